# Optimizing a Trainium2 kernel written in Bass

```python
import math
import jax, jax.numpy as jnp
from jax import lax
import numpy as np

D_MODEL = 2048
BATCH = 4
SEQ = 2048
DEPTH = 2

HEAD_DIM = 128
ROPE_THETA = 10000.0
EPS = 1e-6
RET_HEADS = 8
RET_CHUNK = 128
NSA_HEADS = 8
NSA_KV_HEADS = 2
NSA_GROUP = NSA_HEADS // NSA_KV_HEADS
CMP_BLOCK = 32
CMP_STRIDE = 16
SLC_BLOCK = 64
SLC_TOPK = 16
N_LOCAL_BLOCKS = 2
WINDOW = 512
SLC_Q_BLOCK = 64
WIN_Q_BLOCK = 128
DIFF_HEADS = 8
DIFF_V_DIM = 2 * HEAD_DIM
ATTN_Q_BLOCK = 128

RET_WIDTH = RET_HEADS * HEAD_DIM
NSA_WIDTH = NSA_HEADS * HEAD_DIM
NSA_KV_WIDTH = NSA_KV_HEADS * HEAD_DIM
AB_IN_COLS = 4 * RET_WIDTH + 2 * NSA_WIDTH + 6 * NSA_KV_WIDTH + 3 * NSA_HEADS
AB_OUT_WIDTH = RET_WIDTH + NSA_WIDTH
DIFF_QK_WIDTH = DIFF_HEADS * 2 * HEAD_DIM
DIFF_WIDTH = DIFF_HEADS * DIFF_V_DIM
C_IN_COLS = 2 * DIFF_QK_WIDTH + 2 * DIFF_WIDTH

kernel_name = "hybrid_retention_nsa_diffattn"


def rms_norm(x, g):
    xf = x.astype(jnp.float32)
    y = xf * lax.rsqrt(jnp.mean(xf * xf, axis=-1, keepdims=True) + EPS)
    return (y * g.astype(jnp.float32)).astype(x.dtype)


def head_rms(x):
    xf = x.astype(jnp.float32)
    return (xf * lax.rsqrt(jnp.mean(xf * xf, axis=-1, keepdims=True) + EPS)).astype(x.dtype)


def rope_tables(pos):
    inv = 1.0 / (ROPE_THETA ** (jnp.arange(0, HEAD_DIM, 2, dtype=jnp.float32) / HEAD_DIM))
    ang = pos.astype(jnp.float32)[:, None] * inv[None, :]
    return jnp.cos(ang), jnp.sin(ang)


def apply_rope(x, cos, sin):
    half = x.shape[-1] // 2
    x1, x2 = x[..., :half], x[..., half:]
    c = cos.astype(x.dtype)
    s = sin.astype(x.dtype)
    return jnp.concatenate([x1 * c - x2 * s, x1 * s + x2 * c], axis=-1)


def masked_softmax(s, mask):
    s = jnp.where(mask, s.astype(jnp.float32), -1e30)
    return jax.nn.softmax(s, axis=-1)


def to_heads(t, n_heads):
    B, S, _ = t.shape
    return t.reshape(B, S, n_heads, HEAD_DIM).transpose(0, 2, 1, 3)


def retention(q, k, v):
    B, H, S, Dh = q.shape
    C = RET_CHUNK
    N = S // C
    dt = q.dtype
    log_g = jnp.log1p(-jnp.exp2(-5.0 - jnp.arange(H, dtype=jnp.float32)))
    j = jnp.arange(C, dtype=jnp.float32)
    diff = j[:, None] - j[None, :]
    intra = jnp.where(diff >= 0, jnp.exp(log_g[:, None, None] * jnp.maximum(diff, 0.0)), 0.0)
    q_dec = jnp.exp(log_g[:, None] * (j + 1.0))
    k_dec = jnp.exp(log_g[:, None] * (C - 1.0 - j))
    chunk_dec = jnp.exp(log_g * C)
    k = k * (Dh ** -0.5)
    qc = q.reshape(B, H, N, C, Dh)
    kc = k.reshape(B, H, N, C, Dh)
    vc = v.reshape(B, H, N, C, Dh)
    scores = jnp.einsum('bhnid,bhnjd->bhnij', qc, kc) * intra[:, None].astype(dt)
    inner = jnp.einsum('bhnij,bhnjd->bhnid', scores, vc)
    kv = jnp.einsum('bhnjd,bhnje->bhnde', kc * k_dec[:, None, :, None].astype(dt), vc)

    def step(state, kv_n):
        return state * chunk_dec[None, :, None, None] + kv_n, state

    init = jnp.zeros((B, H, Dh, Dh), jnp.float32)
    _, prev = lax.scan(step, init, jnp.moveaxis(kv, 2, 0).astype(jnp.float32))
    prev = jnp.moveaxis(prev, 0, 2).astype(dt)
    cross = jnp.einsum('bhnid,bhnde->bhnie', qc * q_dec[:, None, :, None].astype(dt), prev)
    return (inner + cross).reshape(B, H, S, Dh)


def nsa_compress(t, pe, w1, w2):
    B, G, S, D = t.shape
    n_cmp = (S - CMP_BLOCK) // CMP_STRIDE + 1
    idx = np.arange(n_cmp)[:, None] * CMP_STRIDE + np.arange(CMP_BLOCK)[None, :]
    blocks = (t[:, :, idx] + pe).reshape(B, G, n_cmp, CMP_BLOCK * D)
    return jax.nn.silu(blocks @ w1) @ w2


def nsa(qg, kc_raw, vc_raw, ks, vs, kw, vw, k_g, pe_k, w1_k, w2_k, pe_v, w1_v, w2_v, cos, sin):
    B, G, R, S, D = qg.shape
    dt = qg.dtype
    scale = HEAD_DIM ** -0.5
    t = jnp.arange(S)
    n_cmp = (S - CMP_BLOCK) // CMP_STRIDE + 1
    cmp_end = jnp.arange(n_cmp) * CMP_STRIDE + CMP_BLOCK - 1
    cos_c, sin_c = rope_tables(cmp_end)
    k_cmp = apply_rope(rms_norm(nsa_compress(kc_raw, pe_k, w1_k, w2_k), k_g), cos_c, sin_c)
    v_cmp = nsa_compress(vc_raw, pe_v, w1_v, w2_v)
    cmask = cmp_end[None, :] <= t[:, None]
    s_cmp = jnp.einsum('bgrsd,bgnd->bgrsn', qg, k_cmp) * scale
    p_cmp = masked_softmax(s_cmp, cmask) * cmask
    o_cmp = jnp.einsum('bgrsn,bgnd->bgrsd', p_cmp.astype(dt), v_cmp)
    n_slc = S // SLC_BLOCK
    c_start = np.arange(n_cmp) * CMP_STRIDE
    s_start = np.arange(n_slc) * SLC_BLOCK
    overlap = jnp.asarray(((c_start[:, None] <= s_start[None, :] + SLC_BLOCK - 1)
                           & (c_start[:, None] + CMP_BLOCK - 1 >= s_start[None, :])).astype(np.float32))
    imp = jnp.einsum('bgrsn,nm->bgsm', p_cmp, overlap)
    blk_t = t // SLC_BLOCK
    jb = jnp.arange(n_slc)
    back = blk_t[:, None] - jb[None, :]
    forced = (jb[None, :] == 0) | ((back >= 0) & (back < N_LOCAL_BLOCKS))
    causal_blk = back >= 0
    score = jnp.where(forced, 1e9, jnp.where(causal_blk, imp, -1e9))
    k_top = min(SLC_TOPK, n_slc)
    _, sel = lax.top_k(score, k_top)
    kb = ks.reshape(B, G, n_slc, SLC_BLOCK, D)
    vb = vs.reshape(B, G, n_slc, SLC_BLOCK, D)
    gather = jax.vmap(jax.vmap(lambda blocks, ids: blocks[ids]))

    def slc_block(q0):
        qb = lax.dynamic_slice_in_dim(qg, q0, SLC_Q_BLOCK, axis=3)
        ib = lax.dynamic_slice_in_dim(sel, q0, SLC_Q_BLOCK, axis=2)
        kg = gather(kb, ib).reshape(B, G, SLC_Q_BLOCK, k_top * SLC_BLOCK, D)
        vg = gather(vb, ib).reshape(B, G, SLC_Q_BLOCK, k_top * SLC_BLOCK, D)
        kpos = (ib[..., None] * SLC_BLOCK + jnp.arange(SLC_BLOCK)).reshape(B, G, SLC_Q_BLOCK, k_top * SLC_BLOCK)
        qpos = q0 + jnp.arange(SLC_Q_BLOCK)
        mask = kpos <= qpos[None, None, :, None]
        s = jnp.einsum('bgrqd,bgqkd->bgrqk', qb, kg) * scale
        p = masked_softmax(s, mask[:, :, None])
        return jnp.einsum('bgrqk,bgqkd->bgrqd', p.astype(dt), vg)

    o_slc = lax.map(slc_block, jnp.arange(0, S, SLC_Q_BLOCK))
    o_slc = jnp.moveaxis(o_slc, 0, 3).reshape(B, G, R, S, D)
    kwp = jnp.pad(kw, ((0, 0), (0, 0), (WINDOW, 0), (0, 0)))
    vwp = jnp.pad(vw, ((0, 0), (0, 0), (WINDOW, 0), (0, 0)))

    def win_block(q0):
        qb = lax.dynamic_slice_in_dim(qg, q0, WIN_Q_BLOCK, axis=3)
        kband = lax.dynamic_slice_in_dim(kwp, q0, WINDOW + WIN_Q_BLOCK, axis=2)
        vband = lax.dynamic_slice_in_dim(vwp, q0, WINDOW + WIN_Q_BLOCK, axis=2)
        kpos = q0 - WINDOW + jnp.arange(WINDOW + WIN_Q_BLOCK)
        qpos = q0 + jnp.arange(WIN_Q_BLOCK)
        d = qpos[:, None] - kpos[None, :]
        mask = (kpos[None, :] >= 0) & (d >= 0) & (d < WINDOW)
        s = jnp.einsum('bgrqd,bgkd->bgrqk', qb, kband) * scale
        p = masked_softmax(s, mask)
        return jnp.einsum('bgrqk,bgkd->bgrqd', p.astype(dt), vband)

    o_win = lax.map(win_block, jnp.arange(0, S, WIN_Q_BLOCK))
    o_win = jnp.moveaxis(o_win, 0, 3).reshape(B, G, R, S, D)
    return o_cmp, o_slc, o_win


def hybrid_ab_layer(x, norm_g, w_in, w_out, nsa_q_g, nsa_k_g, pe_k, w1_k, w2_k, pe_v, w1_v, w2_v, cos, sin):
    B, S, _ = x.shape
    h = rms_norm(x, norm_g)
    proj = h @ w_in
    sizes = [RET_WIDTH] * 4 + [NSA_WIDTH] + [NSA_KV_WIDTH] * 6 + [NSA_WIDTH]
    cuts = [int(c) for c in np.cumsum(sizes)]
    (rq, rk, rv, rgate, nq, kc, vc, ks, vs, kw, vw, ngate, ngl) = jnp.split(proj, cuts, axis=-1)
    y_ret = retention(apply_rope(to_heads(rq, RET_HEADS), cos, sin),
                      apply_rope(to_heads(rk, RET_HEADS), cos, sin),
                      to_heads(rv, RET_HEADS))
    y_ret = head_rms(y_ret).transpose(0, 2, 1, 3).reshape(B, S, RET_WIDTH) * jax.nn.silu(rgate)
    q = apply_rope(rms_norm(to_heads(nq, NSA_HEADS), nsa_q_g), cos, sin)
    qg = q.reshape(B, NSA_KV_HEADS, NSA_GROUP, S, HEAD_DIM)
    kv_heads = lambda t: to_heads(t, NSA_KV_HEADS)
    ks_h = apply_rope(rms_norm(kv_heads(ks), nsa_k_g), cos, sin)
    kw_h = apply_rope(rms_norm(kv_heads(kw), nsa_k_g), cos, sin)
    o_cmp, o_slc, o_win = nsa(qg, kv_heads(kc), kv_heads(vc), ks_h, kv_heads(vs), kw_h, kv_heads(vw),
                              nsa_k_g, pe_k, w1_k, w2_k, pe_v, w1_v, w2_v, cos, sin)
    gates = jax.nn.sigmoid(ngl.astype(jnp.float32)).reshape(B, S, 3, NSA_HEADS)
    gates = gates.transpose(2, 0, 3, 1)[..., None].astype(x.dtype)
    hs = (B, NSA_HEADS, S, HEAD_DIM)
    y_nsa = gates[0] * o_cmp.reshape(hs) + gates[1] * o_slc.reshape(hs) + gates[2] * o_win.reshape(hs)
    y_nsa = y_nsa.transpose(0, 2, 1, 3).reshape(B, S, NSA_WIDTH) * jax.nn.silu(ngate)
    y = jnp.concatenate([y_ret, y_nsa], axis=-1)
    return x + y @ w_out


def diff_layer(x, norm_g, w_in, w_out, q_g, k_g, lq1, lk1, lq2, lk2, lambda_init, cos, sin):
    B, S, _ = x.shape
    dt = x.dtype
    scale = HEAD_DIM ** -0.5
    h = rms_norm(x, norm_g)
    proj = h @ w_in
    q, k, v, gate = jnp.split(proj, [DIFF_QK_WIDTH, 2 * DIFF_QK_WIDTH, 2 * DIFF_QK_WIDTH + DIFF_WIDTH], axis=-1)
    q = q.reshape(B, S, DIFF_HEADS, 2, HEAD_DIM).transpose(0, 2, 3, 1, 4)
    k = k.reshape(B, S, DIFF_HEADS, 2, HEAD_DIM).transpose(0, 2, 3, 1, 4)
    q = apply_rope(rms_norm(q, q_g), cos, sin)
    k = apply_rope(rms_norm(k, k_g), cos, sin)
    v = v.reshape(B, S, DIFF_HEADS, DIFF_V_DIM).transpose(0, 2, 1, 3)
    lam = (jnp.exp(jnp.sum(lq1.astype(jnp.float32) * lk1.astype(jnp.float32)))
           - jnp.exp(jnp.sum(lq2.astype(jnp.float32) * lk2.astype(jnp.float32))) + lambda_init)
    kpos = jnp.arange(S)

    def blk(q0):
        qb = lax.dynamic_slice_in_dim(q, q0, ATTN_Q_BLOCK, axis=3)
        s = jnp.einsum('bhcqd,bhckd->bhcqk', qb, k) * scale
        qpos = q0 + jnp.arange(ATTN_Q_BLOCK)
        p = masked_softmax(s, kpos[None, :] <= qpos[:, None])
        a = p[:, :, 0] - lam * p[:, :, 1]
        return jnp.einsum('bhqk,bhkd->bhqd', a.astype(dt), v)

    o = lax.map(blk, jnp.arange(0, S, ATTN_Q_BLOCK))
    o = jnp.moveaxis(o, 0, 2).reshape(B, DIFF_HEADS, S, DIFF_V_DIM)
    o = head_rms(o) * (1.0 - lambda_init)
    o = o.transpose(0, 2, 1, 3).reshape(B, S, DIFF_WIDTH) * jax.nn.silu(gate)
    return x + o @ w_out


def setup_inputs(seed: int = 0) -> dict:
    key = jax.random.key(seed)
    ks = jax.random.split(key, 24)
    f32 = jnp.float32

    def nrm(k, shape, scale):
        return jax.random.normal(k, shape, f32) * scale

    def gain(k, n):
        return 1.0 + 0.02 * jax.random.normal(k, (n,), f32)

    cin = CMP_BLOCK * HEAD_DIM
    return {
        "x": nrm(ks[0], (BATCH, SEQ, D_MODEL), 1.0),
        "l0_norm_g": gain(ks[1], D_MODEL),
        "l0_w_in": nrm(ks[2], (D_MODEL, AB_IN_COLS), D_MODEL ** -0.5),
        "l0_w_out": nrm(ks[3], (AB_OUT_WIDTH, D_MODEL), AB_OUT_WIDTH ** -0.5),
        "l0_nsa_q_norm_g": gain(ks[4], HEAD_DIM),
        "l0_nsa_k_norm_g": gain(ks[5], HEAD_DIM),
        "l0_cmp_pe_k": nrm(ks[6], (CMP_BLOCK, HEAD_DIM), 0.1),
        "l0_cmp_w1_k": nrm(ks[7], (cin, HEAD_DIM), cin ** -0.5),
        "l0_cmp_w2_k": nrm(ks[8], (HEAD_DIM, HEAD_DIM), HEAD_DIM ** -0.5),
        "l0_cmp_pe_v": nrm(ks[9], (CMP_BLOCK, HEAD_DIM), 0.1),
        "l0_cmp_w1_v": nrm(ks[10], (cin, HEAD_DIM), cin ** -0.5),
        "l0_cmp_w2_v": nrm(ks[11], (HEAD_DIM, HEAD_DIM), HEAD_DIM ** -0.5),
        "l1_norm_g": gain(ks[12], D_MODEL),
        "l1_w_in": nrm(ks[13], (D_MODEL, C_IN_COLS), D_MODEL ** -0.5),
        "l1_w_out": nrm(ks[14], (DIFF_WIDTH, D_MODEL), DIFF_WIDTH ** -0.5),
        "l1_q_norm_g": gain(ks[15], HEAD_DIM),
        "l1_k_norm_g": gain(ks[16], HEAD_DIM),
        "l1_lambda_q1": nrm(ks[17], (HEAD_DIM,), 0.1),
        "l1_lambda_k1": nrm(ks[18], (HEAD_DIM,), 0.1),
        "l1_lambda_q2": nrm(ks[19], (HEAD_DIM,), 0.1),
        "l1_lambda_k2": nrm(ks[20], (HEAD_DIM,), 0.1),
    }


def reference(x, l0_norm_g, l0_w_in, l0_w_out, l0_nsa_q_norm_g, l0_nsa_k_norm_g,
              l0_cmp_pe_k, l0_cmp_w1_k, l0_cmp_w2_k, l0_cmp_pe_v, l0_cmp_w1_v, l0_cmp_w2_v,
              l1_norm_g, l1_w_in, l1_w_out, l1_q_norm_g, l1_k_norm_g,
              l1_lambda_q1, l1_lambda_k1, l1_lambda_q2, l1_lambda_k2):
    S = x.shape[1]
    cos, sin = rope_tables(jnp.arange(S))
    layer_params = [
        (l0_norm_g, l0_w_in, l0_w_out, l0_nsa_q_norm_g, l0_nsa_k_norm_g,
         l0_cmp_pe_k, l0_cmp_w1_k, l0_cmp_w2_k, l0_cmp_pe_v, l0_cmp_w1_v, l0_cmp_w2_v),
        (l1_norm_g, l1_w_in, l1_w_out, l1_q_norm_g, l1_k_norm_g,
         l1_lambda_q1, l1_lambda_k1, l1_lambda_q2, l1_lambda_k2),
    ]
    for i in range(DEPTH):
        if i % 2 == 0:
            x = hybrid_ab_layer(x, *layer_params[i], cos, sin)
        else:
            lambda_init = 0.8 - 0.6 * math.exp(-0.3 * i)
            x = diff_layer(x, *layer_params[i], lambda_init, cos, sin)
    return x
```

```python
import math
from contextlib import ExitStack
import numpy as np
import ml_dtypes
import concourse.bass as bass
import concourse.mybir as mybir
from concourse.bass_utils import run_bass_kernel_spmd

F32 = mybir.dt.float32
BF16 = mybir.dt.bfloat16
AF = mybir.ActivationFunctionType
ALU = mybir.AluOpType
AX = mybir.AxisListType
NPBF = ml_dtypes.bfloat16

S_LEN = 2048
D = 2048
NT = 16
HD = 128
EPS = 1e-6
SCALE = HD ** -0.5
NEG = -30000.0
LAMBDA_INIT = 0.8 - 0.6 * math.exp(-0.3 * 1)
NDMA = 48


class Buf:
    __slots__ = ("name", "w", "r")

    def __init__(self, name=""):
        self.name = name
        self.w = None
        self.r = []


class Sync:
    ENGS = ("pe", "act", "dve", "pool", "sp")

    def __init__(self, nc, es):
        self.nc = nc
        self.sem = {k: es.enter_context(nc.semaphore("s_" + k)) for k in self.ENGS}
        self.cnt = {k: 0 for k in self.ENGS}
        self.seen = {k: {} for k in self.ENGS}
        self.prog = {k: [] for k in self.ENGS}
        self.dq = {"sp": ("d", 28), "pool": ("g", 12)}
        self.dma_val = {}
        self.dma_next = {q: 0 for q in self.dq}
        for q, (pfx, n) in self.dq.items():
            for i in range(n):
                k = "%s%d" % (pfx, i)
                self.sem[k] = es.enter_context(nc.semaphore(k))
                self.dma_val[k] = 0

    def _deps(self, e, reads, writes, extra=()):
        need = {}

        def add(tok):
            if tok is None:
                return
            k, v = tok
            if need.get(k, 0) < v:
                need[k] = v
        for b in reads:
            add(b.w)
        for b in writes:
            add(b.w)
            for t in b.r:
                add(t)
        for t in extra:
            add(t)
        waits = []
        seen = self.seen[e]
        for k, v in need.items():
            if k == e and e == "pe":
                continue
            if seen.get(k, 0) >= v:
                continue
            seen[k] = v
            waits.append((k, v))
        return waits

    def _mark(self, tok, reads, writes):
        for b in reads:
            b.r.append(tok)
            if len(b.r) > 64:
                mx = {}
                for (k, v) in b.r:
                    if mx.get(k, 0) < v:
                        mx[k] = v
                b.r = list(mx.items())
        for b in writes:
            b.w = tok
            b.r = []

    def op(self, e, fn, reads=(), writes=()):
        waits = self._deps(e, reads, writes)
        self.cnt[e] += 1
        tok = (e, self.cnt[e])
        self.prog[e].append((waits, fn, (e, 1)))
        self._mark(tok, reads, writes)
        return tok

    def dma(self, q, fn, reads=(), writes=()):
        pfx, n = self.dq[q]
        i = self.dma_next[q]
        self.dma_next[q] = (i + 1) % n
        k = "%s%d" % (pfx, i)
        extra = []
        if self.dma_val[k] > 0:
            extra.append((k, self.dma_val[k]))
        waits = self._deps(q, reads, writes, extra)
        self.dma_val[k] += 16
        tok = (k, self.dma_val[k])
        self.prog[q].append((waits, fn, (k, 16)))
        self._mark(tok, reads, writes)
        return tok

    def barrier(self):
        toks = [(k, self.cnt[k]) for k in self.ENGS if self.cnt[k] > 0]
        toks += [(k, v) for k, v in self.dma_val.items() if v > 0]
        for e in self.ENGS:
            waits = []
            for (k, v) in toks:
                if k == e and e == "pe":
                    continue
                if self.seen[e].get(k, 0) >= v:
                    continue
                self.seen[e][k] = v
                waits.append((k, v))
            self.prog[e].append((waits, None, None))

    def emit(self):
        nc = self.nc
        sem = self.sem

        def replay(key, eng):
            for waits, fn, inc in self.prog[key]:
                for (k, v) in waits:
                    eng.wait_ge(sem[k], v)
                if fn is None:
                    continue
                ins = fn(eng)
                if inc is not None:
                    ins.then_inc(sem[inc[0]], inc[1])

        with nc.Block() as block:
            @block.tensor
            def _(eng):
                replay("pe", eng)

            @block.scalar
            def _(eng):
                replay("act", eng)

            @block.vector
            def _(eng):
                replay("dve", eng)

            @block.gpsimd
            def _(eng):
                replay("pool", eng)

            @block.sync
            def _(eng):
                replay("sp", eng)


class Ring:
    def __init__(self, tensors):
        self.items = [(t, Buf()) for t in tensors]
        self.i = 0

    def next(self):
        it = self.items[self.i % len(self.items)]
        self.i += 1
        return it


def _rope_np(pos):
    inv = (1.0 / (np.float32(10000.0) ** (np.arange(0, HD, 2, dtype=np.float32) / np.float32(HD)))).astype(np.float32)
    ang = pos.astype(np.float32)[:, None] * inv[None, :]
    return np.cos(ang).astype(np.float32), np.sin(ang).astype(np.float32)


def make_consts(ret_heads):
    c = {}
    c["ident"] = np.eye(128, dtype=np.float32).astype(NPBF)
    cos, sin = _rope_np(np.arange(S_LEN))
    c["cosT"] = np.ascontiguousarray(cos.reshape(NT, 128, 64).transpose(1, 0, 2))
    c["sinT"] = np.ascontiguousarray(sin.reshape(NT, 128, 64).transpose(1, 0, 2))
    cc, sc = _rope_np(np.arange(127) * 16 + 31)
    c["cosc"] = cc
    c["sinc"] = sc
    RH = len(ret_heads)
    j = np.arange(128, dtype=np.float64)
    DT = np.zeros((128, RH, 128), np.float32)
    qdec = np.zeros((128, RH, 128), np.float32)
    kdec = np.zeros((128, RH), np.float32)
    cdec = []
    for li, h in enumerate(ret_heads):
        lg = np.log1p(-np.exp2(-5.0 - h))
        diff = j[None, :] - j[:, None]
        DT[:, li, :] = np.where(diff >= 0, np.exp(lg * np.maximum(diff, 0.0)), 0.0) * SCALE
        qdec[:, li, :] = np.exp(lg * (j + 1.0))[None, :]
        kdec[:, li] = np.exp(lg * (127.0 - j)) * SCALE
        cdec.append(float(np.exp(lg * 128.0)))
    c["DT"] = DT
    c["qdec"] = qdec
    c["kdec"] = kdec
    n = np.arange(127)
    q = np.arange(S_LEN)
    c["cmaskT"] = ((n[:, None] * 16 + 31) <= q[None, :]).astype(np.float32).astype(NPBF)
    c_start = n * 16
    s_start = np.arange(32) * 64
    ov = ((c_start[:, None] <= s_start[None, :] + 63) & (c_start[:, None] + 31 >= s_start[None, :])).astype(np.float32)
    cv = np.zeros((127, 161), np.float32)
    cv[:, 128] = 1.0
    cv[:, 129:161] = ov
    c["cmpv_init"] = cv.astype(NPBF)
    E = np.zeros((32, NT, 128), np.float32)
    for kt in range(NT):
        for kl in range(128):
            E[2 * kt + kl // 64, kt, kl] = -NEG
    c["E"] = E.astype(NPBF)
    kk = np.arange(128)[:, None]
    qq = np.arange(128)[None, :]
    c["tri"] = np.where(kk > qq, NEG, 0.0).astype(np.float32).astype(NPBF)
    c["anti"] = np.where(qq >= kk, NEG, 0.0).astype(np.float32).astype(NPBF)
    M1 = np.zeros((128, NT, 32), np.float32)
    M2 = np.zeros((128, NT, 32), np.float32)
    for t in range(NT):
        for p in range(128):
            blk = 2 * t + p // 64
            for m in range(32):
                back = blk - m
                forced = (m == 0) or (0 <= back < 2)
                causal = back >= 0
                if forced:
                    M2[p, t, m] = 3e9 if m == 0 else (2e9 if back == 0 else 1e9)
                elif causal:
                    M1[p, t, m] = 1.0
                else:
                    M2[p, t, m] = -1e9 - 1e7 * m
    c["M1"] = M1
    c["M2"] = M2
    return c, cdec


def A(fn, **kw):
    return lambda e: getattr(e, fn)(**kw)


def MM(lst):
    def f(e):
        n = len(lst)
        ins = None
        for i, d in enumerate(lst):
            ins = e.matmul(d["out"], lhsT=d["lhsT"], rhs=d["rhs"], start=(i == 0), stop=(i == n - 1))
        return ins
    return f


def MMRAW(lst):
    def f(e):
        ins = None
        for d in lst:
            ins = e.matmul(d["out"], lhsT=d["lhsT"], rhs=d["rhs"], start=d["start"], stop=d["stop"])
        return ins
    return f


def TR(lst):
    def f(e):
        ins = None
        for d in lst:
            ins = e.transpose(out=d["out"], in_=d["in_"], identity=d["identity"])
        return ins
    return f


def build(RH=8, NH=8, NG=2, DH=8, cdec=None, debug=(), stop_after=None, start_at=None, feed=()):
    nc = bass.Bass("TRN2", target_bir_lowering=False)
    GQ = NH // NG
    NC0 = 4 * RH * 128 + 2 * NH * 128 + 6 * NG * 128 + 3 * NH
    NC1 = 8 * DH * 128

    def din(name, shape, dt=F32):
        return nc.dram_tensor(name, list(shape), dt, kind="ExternalInput").ap()

    x_in = din("x", [S_LEN, D])
    w_in0 = din("w_in0", [D, NC0])
    w_out0 = din("w_out0", [D, D])
    w_in1 = din("w_in1", [D, NC1])
    w_out1 = din("w_out1", [D, D])
    g_norm0 = din("g_norm0", [128, D])
    g_norm1 = din("g_norm1", [128, D])
    g_q0 = din("g_q0", [128, 128])
    g_k0 = din("g_k0", [128, 128])
    g_q1 = din("g_q1", [128, 128])
    g_k1 = din("g_k1", [128, 128])
    lam_in = din("lam_in", [128, 4, 128])
    w1_in = {"k": din("w1k", [4096, 128]), "v": din("w1v", [4096, 128])}
    w2_in = {"k": din("w2k", [128, 128]), "v": din("w2v", [128, 128])}
    pe_in = {"k": din("pek", [128, 32]), "v": din("pev", [128, 32])}
    c_ident = din("ident", [128, 128], BF16)
    c_cosT = din("cosT", [128, NT, 64])
    c_sinT = din("sinT", [128, NT, 64])
    c_cosc = din("cosc", [127, 64])
    c_sinc = din("sinc", [127, 64])
    c_DT = din("DT", [128, RH, 128])
    c_qdec = din("qdec", [128, RH, 128])
    c_kdec = din("kdec", [128, RH])
    c_cmaskT = din("cmaskT", [127, S_LEN], BF16)
    c_cmpv = din("cmpv_init", [127, 161], BF16)
    c_E = din("E", [32, NT, 128], BF16)
    c_tri = din("tri", [128, 128], BF16)
    c_anti = din("anti", [128, 128], BF16)
    c_M1 = din("M1", [128, NT, 32])
    c_M2 = din("M2", [128, NT, 32])

    out_t = nc.dram_tensor("out", [NT, 128, D], F32, kind="ExternalOutput").ap()

    def scratch(name, shape, dt=BF16):
        kind = "ExternalOutput" if name in debug else ("ExternalInput" if name in feed else "Internal")
        return nc.dram_tensor(name, list(shape), dt, kind=kind).ap()

    FT = {}
    TK = {}
    for nm, nh in (("QrT", RH), ("KrT", RH), ("QnT", NH), ("KcT", NG), ("VcT", NG), ("KsT", NG), ("KwT", NG),
                   ("Q1T", 2 * DH), ("K1T", 2 * DH)):
        FT[nm] = scratch(nm, [nh, 128, S_LEN])
    for nm, ncol in (("Kd", RH * 128), ("Vr", RH * 128), ("Rg", RH * 128), ("Vs", NG * 128), ("Vw", NG * 128),
                     ("Ng", NH * 128), ("V1", DH * 256), ("G1", DH * 256), ("Y0", D), ("Y1", D)):
        TK[nm] = scratch(nm, [NT, 128, ncol])
    TK["Gl"] = scratch("Gl", [NT, 128, 3 * NH], F32)
    TK["X1"] = scratch("X1", [NT, 128, D], F32)
    DB = {k: Buf(k) for k in list(FT) + list(TK)}
    out_buf = Buf("out")
    x_tiles = x_in.rearrange("(t p) d -> t p d", p=128)

    with ExitStack() as es:
        S = Sync(nc, es)

        uid = [0]

        def uname(n):
            uid[0] += 1
            return "%s_u%d" % (n, uid[0])

        def gsb(name, shape, dt):
            return es.enter_context(nc.sbuf_tensor(uname(name), list(shape), dt))
        ident = gsb("ident", [128, 128], BF16)
        cosT = gsb("cosT", [128, NT, 64], F32)
        sinT = gsb("sinT", [128, NT, 64], F32)
        b_const = Buf("const")
        S.dma("sp", A("dma_start", out=ident[:], in_=c_ident[:, :]), writes=[b_const])
        S.dma("sp", A("dma_start", out=cosT[:], in_=c_cosT[:, :, :]), writes=[b_const])
        S.dma("sp", A("dma_start", out=sinT[:], in_=c_sinT[:, :, :]), writes=[b_const])

        def rstd_ops(ss_ap, out_ap, b_ss, b_out, inv_n):
            S.op("dve", A("tensor_scalar", out=out_ap, in0=ss_ap, scalar1=inv_n, scalar2=EPS,
                          op0=ALU.mult, op1=ALU.add), reads=[b_ss], writes=[b_out])
            S.op("act", A("sqrt", out=out_ap, in_=out_ap), reads=[b_out], writes=[b_out])
            S.op("dve", A("reciprocal", out=out_ap, in_=out_ap), reads=[b_out], writes=[b_out])

        def rope_ops(xv, rv, b_xs, b_rb, cb, sbb, b_tab, tmps, P, n):
            (t1, b1), (t2, b2), (t3, b3), (t4, b4) = tmps
            v = lambda tt: tt[0:P, 0:n * 64].rearrange("p (h d) -> p h d", d=64)
            S.op("dve", A("tensor_tensor", out=v(t1), in0=xv[:, :, 0:64], in1=cb, op=ALU.mult),
                 reads=[b_xs, b_tab], writes=[b1])
            S.op("dve", A("tensor_tensor", out=v(t2), in0=xv[:, :, 64:128], in1=sbb, op=ALU.mult),
                 reads=[b_xs, b_tab], writes=[b2])
            S.op("dve", A("tensor_tensor", out=rv[:, :, 0:64], in0=v(t1), in1=v(t2), op=ALU.subtract),
                 reads=[b1, b2], writes=[b_rb])
            S.op("pool", A("tensor_tensor", out=v(t3), in0=xv[:, :, 0:64], in1=sbb, op=ALU.mult),
                 reads=[b_xs, b_tab], writes=[b3])
            S.op("pool", A("tensor_tensor", out=v(t4), in0=xv[:, :, 64:128], in1=cb, op=ALU.mult),
                 reads=[b_xs, b_tab], writes=[b4])
            S.op("pool", A("tensor_tensor", out=rv[:, :, 64:128], in0=v(t3), in1=v(t4), op=ALU.add),
                 reads=[b3, b4], writes=[b_rb])

        def phase_norm(src_tiles, src_buf, g_dram, hT, hT_b):
            with ExitStack() as ps_:
                sb = lambda n, s, d: ps_.enter_context(nc.sbuf_tensor(uname(n), list(s), d))
                pp = lambda n, s, d: ps_.enter_context(nc.psum_tensor(uname(n), list(s), d))
                gt = sb("n_g", [128, D], F32)
                b_g = Buf()
                S.dma("sp", A("dma_start", out=gt[:], in_=g_dram[:, :]), writes=[b_g])
                xr = Ring([sb("n_x%d" % i, [128, D], F32) for i in range(2)])
                junk = sb("n_junk", [128, D], BF16)
                b_junk = Buf()
                ssr = Ring([sb("n_ss%d" % i, [128, 1], F32) for i in range(2)])
                rsr = Ring([sb("n_rs%d" % i, [128, 1], F32) for i in range(2)])
                hbr = Ring([sb("n_hb%d" % i, [128, D], BF16) for i in range(2)])
                ptr = Ring([pp("n_pt%d" % i, [128, 8, 128], BF16) for i in range(2)])
                for t in range(NT):
                    xt, b_x = xr.next()
                    S.dma("sp", A("dma_start", out=xt[:], in_=src_tiles[t]),
                          reads=[src_buf] if src_buf is not None else [], writes=[b_x])
                    ss, b_ss = ssr.next()
                    rs, b_rs = rsr.next()
                    S.op("act", A("activation", out=junk[:], in_=xt[:], func=AF.Square, accum_out=ss[:, 0:1]),
                         reads=[b_x], writes=[b_junk, b_ss])
                    rstd_ops(ss[:], rs[:], b_ss, b_rs, 1.0 / D)
                    hb, b_hb = hbr.next()
                    S.op("dve", A("scalar_tensor_tensor", out=hb[:], in0=xt[:], scalar=rs[:, 0:1], in1=gt[:],
                                  op0=ALU.mult, op1=ALU.mult), reads=[b_x, b_rs, b_g], writes=[b_hb])
                    for half in range(2):
                        pt, b_pt = ptr.next()
                        S.op("pe", TR([dict(out=pt[:, jj, :], in_=hb[:, (half * 8 + jj) * 128:(half * 8 + jj + 1) * 128],
                                            identity=ident[:]) for jj in range(8)]),
                             reads=[b_hb, b_const], writes=[b_pt])
                        dst = hT[:, half * 8:(half + 1) * 8, t * 128:(t + 1) * 128]
                        if half == 0:
                            S.op("act", A("copy", out=dst, in_=pt[:]), reads=[b_pt], writes=[hT_b[t]])
                        else:
                            S.op("dve", A("tensor_copy", out=dst, in_=pt[:]), reads=[b_pt], writes=[hT_b[t]])
            S.barrier()

        def phase_inproj(w_dram, ncols, sections, hT, hT_b, gq_dram, gk_dram, kdec_dram):
            segs = []
            col = 0
            for si, sec in enumerate(sections):
                w_ = sec["ncols"]
                if w_ % 128 == 0:
                    for hh in range(w_ // 128):
                        segs.append((col + hh * 128, 128, si, hh))
                else:
                    segs.append((col, w_, si, 0))
                col += w_
            assert col == ncols
            blocks = []
            cur = []
            for sg in segs:
                if cur and (sg[0] + sg[1] - cur[0][0] > 512):
                    blocks.append(cur)
                    cur = []
                cur.append(sg)
            if cur:
                blocks.append(cur)

            with ExitStack() as ps_:
                sb = lambda n, s, d: ps_.enter_context(nc.sbuf_tensor(uname(n), list(s), d))
                pp = lambda n, s, d: ps_.enter_context(nc.psum_tensor(uname(n), list(s), d))
                wr = Ring([sb("a_w%d" % i, [128, 16, 512], BF16) for i in range(2)])
                stT = sb("a_stT", [128, 4, S_LEN], BF16)
                stK = sb("a_stK", [128, NT, 512], BF16)
                stG = sb("a_stG", [128, NT, 32], F32)
                stT_b = [[Buf() for _ in range(4)] for _ in range(4)]
                stK_b = [[Buf() for _ in range(4)] for _ in range(4)]
                stG_b = [Buf() for _ in range(4)]
                gq = sb("a_gq", [128, 128], F32)
                gk = sb("a_gk", [128, 128], F32)
                b_gain = Buf()
                S.dma("sp", A("dma_start", out=gq[:], in_=gq_dram[:, :]), writes=[b_gain])
                S.dma("sp", A("dma_start", out=gk[:], in_=gk_dram[:, :]), writes=[b_gain])
                kdec = None
                if kdec_dram is not None:
                    kdec = sb("a_kdec", [128, RH], F32)
                    S.dma("sp", A("dma_start", out=kdec[:], in_=kdec_dram[:, :]), writes=[b_gain])
                psr = Ring([pp("a_ps%d" % i, [128, 512], F32) for i in range(2)])
                ptr = Ring([pp("a_pt%d" % i, [128, 4, 128], BF16) for i in range(2)])
                xsr = Ring([sb("a_xs%d" % i, [128, 512], F32) for i in range(2)])
                sqr = Ring([sb("a_sq%d" % i, [128, 512], F32) for i in range(2)])
                msr = Ring([sb("a_ms%d" % i, [128, 4], F32) for i in range(2)])
                rsr = Ring([sb("a_rs%d" % i, [128, 4], F32) for i in range(2)])
                tmr = [Ring([sb("a_t%d_%d" % (k, i), [128, 256], F32) for i in range(2)]) for k in range(4)]
                rbr = Ring([sb("a_rb%d" % i, [128, 512], BF16) for i in range(2)])

                def load_w(bi):
                    blk = blocks[bi]
                    c0 = blk[0][0]
                    wd = blk[-1][0] + blk[-1][1] - c0
                    wt, b_w = wr.next()
                    for hf in range(2):
                        S.dma("pool", A("dma_start", out=wt[:, hf * 8:(hf + 1) * 8, 0:wd],
                                        in_=w_dram[hf * 1024:(hf + 1) * 1024, c0:c0 + wd].rearrange("(k p) n -> p k n", p=128)),
                              writes=[b_w])
                    return wt, b_w, c0, wd

                def do_mm(t, wt, b_w, wd):
                    ps, b_ps = psr.next()
                    S.op("pe", MM([dict(out=ps[:, 0:wd], lhsT=hT[:, k, t * 128:(t + 1) * 128], rhs=wt[:, k, 0:wd])
                                   for k in range(16)]), reads=[hT_b[t], b_w], writes=[b_ps])
                    return ps, b_ps

                def to_T(rb, b_rb, n, t, slot0):
                    pt, b_pt = ptr.next()
                    S.op("pe", TR([dict(out=pt[:, jj, :], in_=rb[:, jj * 128:(jj + 1) * 128], identity=ident[:])
                                   for jj in range(n)]), reads=[b_rb, b_const], writes=[b_pt])
                    S.op("act", A("copy", out=stT[:, slot0:slot0 + n, t * 128:(t + 1) * 128], in_=pt[:, 0:n, :]),
                         reads=[b_pt], writes=[stT_b[t // 4][s_] for s_ in range(slot0, slot0 + n)])

                def post(bi, t, c0, ps, b_ps):
                    blk = blocks[bi]
                    runs = []
                    for sg in blk:
                        if runs and runs[-1][0] == sg[2]:
                            runs[-1][1].append(sg)
                        else:
                            runs.append((sg[2], [sg]))
                    tg = t // 4
                    for si, sgs in runs:
                        sec = sections[si]
                        kind = sec["kind"]
                        lc0 = sgs[0][0] - c0
                        n = len(sgs)
                        w_ = sum(s_[1] for s_ in sgs)
                        slot0 = lc0 // 128
                        h0 = sgs[0][3]
                        kb = [stK_b[tg][s_] for s_ in range(slot0, slot0 + n)]
                        tb = [stT_b[tg][s_] for s_ in range(slot0, slot0 + n)]
                        if kind in ("plain", "silu"):
                            fn = AF.Copy if kind == "plain" else AF.Silu
                            S.op("act", A("activation", out=stK[:, t, lc0:lc0 + w_], in_=ps[:, lc0:lc0 + w_], func=fn),
                                 reads=[b_ps], writes=kb)
                        elif kind == "sigmoid":
                            S.op("act", A("activation", out=stG[:, t, 0:w_], in_=ps[:, lc0:lc0 + w_], func=AF.Sigmoid),
                                 reads=[b_ps], writes=[stG_b[tg]])
                        elif kind == "plainT":
                            rb, b_rb = rbr.next()
                            S.op("act", A("copy", out=rb[:, 0:w_], in_=ps[:, lc0:lc0 + w_]), reads=[b_ps], writes=[b_rb])
                            to_T(rb, b_rb, n, t, slot0)
                        elif kind in ("rope", "rope_k", "normrope_q", "normrope_k"):
                            xs, b_xs = xsr.next()
                            S.op("act", A("copy", out=xs[:, 0:w_], in_=ps[:, lc0:lc0 + w_]), reads=[b_ps], writes=[b_xs])
                            if kind.startswith("normrope"):
                                g_t = gq if kind == "normrope_q" else gk
                                sq, b_sq = sqr.next()
                                ms, b_ms = msr.next()
                                rs, b_rs = rsr.next()
                                S.op("act", A("activation", out=sq[:, 0:w_], in_=ps[:, lc0:lc0 + w_], func=AF.Square),
                                     reads=[b_ps], writes=[b_sq])
                                S.op("dve", A("tensor_reduce", out=ms[:, 0:n],
                                              in_=sq[:, 0:n * 128].rearrange("p (h d) -> p h d", d=128),
                                              axis=AX.X, op=ALU.add), reads=[b_sq], writes=[b_ms])
                                rstd_ops(ms[:, 0:n], rs[:, 0:n], b_ms, b_rs, 1.0 / 128)
                                for jj in range(n):
                                    S.op("dve", A("scalar_tensor_tensor", out=xs[:, jj * 128:(jj + 1) * 128],
                                                  in0=xs[:, jj * 128:(jj + 1) * 128], scalar=rs[:, jj:jj + 1], in1=g_t[:],
                                                  op0=ALU.mult, op1=ALU.mult),
                                         reads=[b_xs, b_rs, b_gain], writes=[b_xs])
                            rb, b_rb = rbr.next()
                            xv = xs[:, 0:n * 128].rearrange("p (h d) -> p h d", d=128)
                            rv = rb[:, 0:n * 128].rearrange("p (h d) -> p h d", d=128)
                            cb = cosT[:, t, :].unsqueeze(1).to_broadcast([128, n, 64])
                            sbb = sinT[:, t, :].unsqueeze(1).to_broadcast([128, n, 64])
                            rope_ops(xv, rv, b_xs, b_rb, cb, sbb, b_const, [r_.next() for r_ in tmr], 128, n)
                            to_T(rb, b_rb, n, t, slot0)
                            if kind == "rope_k":
                                S.op("pool", A("tensor_tensor",
                                               out=stK[:, t, lc0:lc0 + n * 128].rearrange("p (h d) -> p h d", d=128),
                                               in0=rv, in1=kdec[:, h0:h0 + n].unsqueeze(2).to_broadcast([128, n, 128]),
                                               op=ALU.mult), reads=[b_rb, b_gain], writes=kb)
                        else:
                            raise ValueError(kind)
                        if t % 4 == 3:
                            tok0 = (t - 3) * 128
                            if kind in ("rope", "rope_k", "normrope_q", "normrope_k", "plainT"):
                                dst = FT[sec["ft"]]
                                S.dma("sp", A("dma_start",
                                              out=dst[h0:h0 + n, :, tok0:tok0 + 512].rearrange("h d s -> d h s"),
                                              in_=stT[:, slot0:slot0 + n, tok0:tok0 + 512]),
                                      reads=tb, writes=[DB[sec["ft"]]])
                            if kind in ("plain", "silu", "rope_k"):
                                dst = TK[sec["tk"]]
                                cc0 = h0 * 128
                                S.dma("sp", A("dma_start",
                                              out=dst[t - 3:t + 1, :, cc0:cc0 + w_].rearrange("t p c -> p t c"),
                                              in_=stK[:, t - 3:t + 1, lc0:lc0 + w_]),
                                      reads=kb, writes=[DB[sec["tk"]]])
                            if kind == "sigmoid":
                                dst = TK[sec["tk"]]
                                S.dma("sp", A("dma_start",
                                              out=dst[t - 3:t + 1, :, 0:w_].rearrange("t p c -> p t c"),
                                              in_=stG[:, t - 3:t + 1, 0:w_]),
                                      reads=[stG_b[tg]], writes=[DB[sec["tk"]]])

                nb = len(blocks)
                wts = {0: load_w(0)}
                prev = None
                for bi in range(nb):
                    wt, b_w, c0, wd = wts[bi]
                    for t in range(NT):
                        if t == 0 and bi + 1 < nb:
                            wts[bi + 1] = load_w(bi + 1)
                        ps, b_ps = do_mm(t, wt, b_w, wd)
                        if prev is not None:
                            post(*prev)
                        prev = (bi, t, c0, ps, b_ps)
                post(*prev)
            S.barrier()

        def phase_ret():
            GH = min(4, RH)
            with ExitStack() as ps_:
                sb = lambda n, s, d: ps_.enter_context(nc.sbuf_tensor(uname(n), list(s), d))
                pp = lambda n, s, d: ps_.enter_context(nc.psum_tensor(uname(n), list(s), d))
                DT = sb("r_DT", [128, RH, 128], F32)
                qdec = sb("r_qdec", [128, RH, 128], F32)
                b_rc = Buf()
                S.dma("sp", A("dma_start", out=DT[:], in_=c_DT[:, :, :]), writes=[b_rc])
                S.dma("sp", A("dma_start", out=qdec[:], in_=c_qdec[:, :, :]), writes=[b_rc])
                mk = lambda nm, shp, dt: [(sb("r_%s%d" % (nm, i), shp, dt), Buf()) for i in range(GH)]
                QT = mk("QT", [128, S_LEN], BF16)
                KT = mk("KT", [128, S_LEN], BF16)
                Qd = mk("Qd", [128, S_LEN], BF16)
                Kd = mk("Kd", [128, NT, 128], BF16)
                V = mk("V", [128, NT, 128], BF16)
                Rg = mk("Rg", [128, NT, 128], BF16)
                st = mk("st", [128, 128], F32)
                stb = mk("stb", [128, 128], BF16)
                ybuf = sb("r_y", [128, NT, GH * 128], BF16)
                b_y = Buf()
                junk = sb("r_junk", [128, 128], BF16)
                b_junk = Buf()
                sbank = Ring([pp("r_ps%d" % i, [128, 4, 128], F32) for i in range(2)])
                obank = Ring([pp("r_po%d" % i, [128, 4, 128], F32) for i in range(2)])
                kbank = Ring([pp("r_pk%d" % i, [128, 4, 128], F32) for i in range(2)])
                wring = Ring([sb("r_w%d" % i, [128, 4, 128], BF16) for i in range(2)])
                ssr = Ring([sb("r_ss%d" % i, [128, 4], F32) for i in range(2)])
                rsr = Ring([sb("r_rs%d" % i, [128, 4], F32) for i in range(2)])
                for g0 in range(0, RH, GH):
                    heads = list(range(g0, min(RH, g0 + GH)))
                    for i, h in enumerate(heads):
                        S.dma("sp", A("dma_start", out=QT[i][0][:], in_=FT["QrT"][h]), reads=[DB["QrT"]], writes=[QT[i][1]])
                        S.dma("sp", A("dma_start", out=KT[i][0][:], in_=FT["KrT"][h]), reads=[DB["KrT"]], writes=[KT[i][1]])
                        for (dstl, nm) in ((Kd, "Kd"), (V, "Vr"), (Rg, "Rg")):
                            S.dma("sp", A("dma_start", out=dstl[i][0][:],
                                          in_=TK[nm][:, :, h * 128:(h + 1) * 128].rearrange("t p c -> p t c")),
                                  reads=[DB[nm]], writes=[dstl[i][1]])
                        S.op("dve", A("tensor_tensor", out=Qd[i][0][:].rearrange("p (n c) -> p n c", c=128),
                                      in0=QT[i][0][:].rearrange("p (n c) -> p n c", c=128),
                                      in1=qdec[:, h, :].unsqueeze(1).to_broadcast([128, NT, 128]), op=ALU.mult),
                             reads=[QT[i][1], b_rc], writes=[Qd[i][1]])
                    nh_ = len(heads)
                    for n in range(NT):
                        cs = slice(n * 128, (n + 1) * 128)
                        ps_s, b_s = sbank.next()
                        S.op("pe", MMRAW([dict(out=ps_s[:, i, :], lhsT=KT[i][0][:, cs], rhs=QT[i][0][:, cs], start=True, stop=True)
                                          for i in range(nh_)]),
                             reads=[KT[i][1] for i in range(nh_)] + [QT[i][1] for i in range(nh_)], writes=[b_s])
                        wT, b_wT = wring.next()
                        for i, h in enumerate(heads):
                            S.op("dve", A("tensor_tensor", out=wT[:, i, :], in0=ps_s[:, i, :], in1=DT[:, h, :], op=ALU.mult),
                                 reads=[b_s, b_rc], writes=[b_wT])
                        ps_o, b_o = obank.next()
                        lst = []
                        rd = [b_wT]
                        for i in range(nh_):
                            lst.append(dict(out=ps_o[:, i, :], lhsT=wT[:, i, :], rhs=V[i][0][:, n, :], start=True, stop=(n == 0)))
                            rd.append(V[i][1])
                            if n > 0:
                                lst.append(dict(out=ps_o[:, i, :], lhsT=Qd[i][0][:, cs], rhs=stb[i][0][:], start=False, stop=True))
                                rd += [Qd[i][1], stb[i][1]]
                        S.op("pe", MMRAW(lst), reads=rd, writes=[b_o])
                        if n < NT - 1:
                            ps_k, b_k = kbank.next()
                            S.op("pe", MMRAW([dict(out=ps_k[:, i, :], lhsT=Kd[i][0][:, n, :], rhs=V[i][0][:, n, :], start=True, stop=True)
                                              for i in range(nh_)]),
                                 reads=[Kd[i][1] for i in range(nh_)] + [V[i][1] for i in range(nh_)], writes=[b_k])
                            for i, h in enumerate(heads):
                                if n == 0:
                                    S.op("dve", A("tensor_copy", out=st[i][0][:], in_=ps_k[:, i, :]), reads=[b_k], writes=[st[i][1]])
                                else:
                                    S.op("dve", A("scalar_tensor_tensor", out=st[i][0][:], in0=st[i][0][:], scalar=float(cdec[h]),
                                                  in1=ps_k[:, i, :], op0=ALU.mult, op1=ALU.add),
                                         reads=[b_k, st[i][1]], writes=[st[i][1]])
                                S.op("act", A("copy", out=stb[i][0][:], in_=st[i][0][:]), reads=[st[i][1]], writes=[stb[i][1]])
                        ss, b_ss = ssr.next()
                        rs, b_rs = rsr.next()
                        for i in range(nh_):
                            S.op("act", A("activation", out=junk[:], in_=ps_o[:, i, :], func=AF.Square, accum_out=ss[:, i:i + 1]),
                                 reads=[b_o], writes=[b_junk, b_ss])
                        rstd_ops(ss[:, 0:nh_], rs[:, 0:nh_], b_ss, b_rs, 1.0 / 128)
                        for i in range(nh_):
                            S.op("dve", A("scalar_tensor_tensor", out=ybuf[:, n, i * 128:(i + 1) * 128], in0=ps_o[:, i, :],
                                          scalar=rs[:, i:i + 1], in1=Rg[i][0][:, n, :], op0=ALU.mult, op1=ALU.mult),
                                 reads=[b_o, b_rs, Rg[i][1]], writes=[b_y])
                    S.dma("sp", A("dma_start",
                                  out=TK["Y0"][:, :, g0 * 128:(g0 + nh_) * 128].rearrange("t p c -> p t c"),
                                  in_=ybuf[:, :, 0:nh_ * 128]), reads=[b_y], writes=[DB["Y0"]])
            S.barrier()

        def attention(mode, QT, b_QT, KT, b_KT, V_of, b_V, dv1, rings, finalize, extra=None):
            sring, pring, accs, E, selT, b_sel, tri, anti, b_msk = rings
            for c in range(4):
                kts = range(max(0, 4 * c - 4), 4 * c + 4) if mode == "win" else range(0, 4 * c + 4)
                for kt in kts:
                    lo = max(4 * c, kt)
                    hi = min(4 * c + 3, kt + 4) if mode == "win" else 4 * c + 3
                    cl, ch = (lo - 4 * c) * 128, (hi - 4 * c + 1) * 128
                    q0 = 4 * c * 128
                    ps_s, b_s = sring.next()
                    lst = [dict(out=ps_s[:, cl:ch], lhsT=KT[:, kt * 128:(kt + 1) * 128], rhs=QT[:, q0 + cl:q0 + ch])]
                    rd = [b_KT, b_QT, b_msk, b_const]
                    if mode == "slc":
                        lst.append(dict(out=ps_s[:, cl:ch], lhsT=E[:, kt, :], rhs=selT[:, q0 + cl:q0 + ch]))
                        rd.append(b_sel)
                    if lo == kt:
                        d0 = (kt - 4 * c) * 128
                        lst.append(dict(out=ps_s[:, d0:d0 + 128], lhsT=ident[:], rhs=tri[:]))
                    if mode == "win" and lo <= kt + 4 <= hi:
                        d0 = (kt + 4 - 4 * c) * 128
                        lst.append(dict(out=ps_s[:, d0:d0 + 128], lhsT=ident[:], rhs=anti[:]))
                    S.op("pe", MM(lst), reads=rd, writes=[b_s])
                    pT, b_pT = pring.next()
                    S.op("act", A("activation", out=pT[:, cl:ch], in_=ps_s[:, cl:ch], func=AF.Exp, scale=SCALE),
                         reads=[b_s], writes=[b_pT])
                    for qt in range(lo, hi + 1):
                        acc, b_acc = accs[qt % 4]
                        first = (kt == (max(0, qt - 4) if mode == "win" else 0))
                        last = (kt == qt)
                        d0 = (qt - 4 * c) * 128
                        S.op("pe", MMRAW([dict(out=acc[:, 0:dv1], lhsT=pT[:, d0:d0 + 128], rhs=V_of(kt), start=first, stop=last)]),
                             reads=[b_pT, b_V], writes=[b_acc])
                        if last:
                            finalize(qt, acc, b_acc)

        def phase_nsa():
            with ExitStack() as ps_:
                sb = lambda n, s, d: ps_.enter_context(nc.sbuf_tensor(uname(n), list(s), d))
                pp = lambda n, s, d: ps_.enter_context(nc.psum_tensor(uname(n), list(s), d))
                b_k = Buf()
                cmaskT = sb("s_cmask", [127, S_LEN], BF16)
                E = sb("s_E", [32, NT, 128], BF16)
                tri = sb("s_tri", [128, 128], BF16)
                anti = sb("s_anti", [128, 128], BF16)
                M1 = sb("s_M1", [128, NT, 32], F32)
                M2 = sb("s_M2", [128, NT, 32], F32)
                cosc = sb("s_cosc", [127, 64], F32)
                sinc = sb("s_sinc", [127, 64], F32)
                gk = sb("s_gk", [128, 128], F32)
                Gl = sb("s_Gl", [128, NT, 3 * NH], F32)
                for (dst, src) in ((cmaskT, c_cmaskT[:, :]), (E, c_E[:, :, :]), (tri, c_tri[:, :]), (anti, c_anti[:, :]),
                                   (M1, c_M1[:, :, :]), (M2, c_M2[:, :, :]), (cosc, c_cosc[:, :]), (sinc, c_sinc[:, :]),
                                   (gk, g_k0[:, :])):
                    S.dma("sp", A("dma_start", out=dst[:], in_=src), writes=[b_k])
                S.dma("sp", A("dma_start", out=Gl[:], in_=TK["Gl"].rearrange("t p c -> p t c")), reads=[DB["Gl"]], writes=[b_k])
                W1 = {}
                W2 = {}
                peT = {}
                for kv in ("k", "v"):
                    W1[kv] = sb("s_W1" + kv, [128, 32, 128], BF16)
                    W2[kv] = sb("s_W2" + kv, [128, 128], BF16)
                    peT[kv] = sb("s_pe" + kv, [128, 32], BF16)
                    S.dma("pool", A("dma_start", out=W1[kv][:], in_=w1_in[kv].rearrange("(j d) f -> d j f", d=128)), writes=[b_k])
                    S.dma("pool", A("dma_start", out=W2[kv][:], in_=w2_in[kv][:, :]), writes=[b_k])
                    S.dma("pool", A("dma_start", out=peT[kv][:], in_=pe_in[kv][:, :]), writes=[b_k])
                XcT = {"k": (sb("s_KcT", [128, S_LEN], BF16), Buf()), "v": (sb("s_VcT", [128, S_LEN], BF16), Buf())}
                QTs = [(sb("s_QT%d" % i, [128, S_LEN], BF16), Buf()) for i in range(GQ)]
                KsT = (sb("s_KsT", [128, S_LEN], BF16), Buf())
                KwT = (sb("s_KwT", [128, S_LEN], BF16), Buf())
                Vs = (sb("s_Vs", [128, NT, 129], BF16), Buf())
                Vw = (sb("s_Vw", [128, NT, 129], BF16), Buf())
                S.op("dve", A("memset", ap=Vs[0][:, :, 128:129], constant=1.0), writes=[Vs[1]])
                S.op("dve", A("memset", ap=Vw[0][:, :, 128:129], constant=1.0), writes=[Vw[1]])
                Ng = (sb("s_Ng", [128, NT, GQ * 128], BF16), Buf())
                nacc = sb("s_nacc", [128, NT, GQ, 128], F32)
                nacc_b = [[Buf() for _ in range(GQ)] for _ in range(NT)]
                imp = sb("s_imp", [128, NT, 32], F32)
                imp_b = [Buf() for _ in range(NT)]
                selT = sb("s_selT", [32, S_LEN], BF16)
                b_sel = Buf()
                ybuf = sb("s_y", [128, NT, GQ * 128], BF16)
                b_y = Buf()
                kcmpT = (sb("s_kcmpT", [128, 128], BF16), Buf())
                cmpV = (sb("s_cmpV", [127, 161], BF16), Buf())
                cb = (sb("s_cb", [128, 1], F32), Buf())
                hT_ = (sb("s_hT", [128, 128], BF16), Buf())
                xs = (sb("s_xs", [127, 128], F32), Buf())
                kc = (sb("s_kc", [127, 128], BF16), Buf())
                junk = (sb("s_junk", [127, 128], BF16), Buf())
                ms = (sb("s_ms", [127, 1], F32), Buf())
                rs1 = (sb("s_rs1", [127, 1], F32), Buf())
                tmps = [(sb("s_t%d" % i, [127, 64], F32), Buf()) for i in range(4)]
                sring = Ring([pp("s_ps%d" % i, [128, 512], F32) for i in range(2)])
                accs = [(pp("s_acc%d" % i, [128, 512], F32), Buf()) for i in range(4)]
                mring = Ring([pp("s_pm%d" % i, [128, 512], F32) for i in range(2)])
                pring = Ring([sb("s_pT%d" % i, [128, 512], BF16) for i in range(3)])
                rsr = Ring([sb("s_rs%d" % i, [128, 1], F32) for i in range(4)])
                cfr = Ring([sb("s_cf%d" % i, [128, 1], F32) for i in range(4)])
                scr = Ring([sb("s_sc%d" % i, [128, 32], F32) for i in range(2)])
                sc2r = Ring([sb("s_sc2%d" % i, [128, 32], F32) for i in range(2)])
                m8r = Ring([sb("s_m8%d" % i, [128, 16], F32) for i in range(2)])
                sbr = Ring([sb("s_sb%d" % i, [128, 32], BF16) for i in range(2)])
                rings = (sring, pring, accs, E, selT, b_sel, tri, anti, b_k)

                for g in range(NG):
                    S.dma("sp", A("dma_start", out=XcT["k"][0][:], in_=FT["KcT"][g]), reads=[DB["KcT"]], writes=[XcT["k"][1]])
                    S.dma("sp", A("dma_start", out=XcT["v"][0][:], in_=FT["VcT"][g]), reads=[DB["VcT"]], writes=[XcT["v"][1]])
                    for r in range(GQ):
                        S.dma("sp", A("dma_start", out=QTs[r][0][:], in_=FT["QnT"][g * GQ + r]), reads=[DB["QnT"]], writes=[QTs[r][1]])
                    S.dma("sp", A("dma_start", out=KsT[0][:], in_=FT["KsT"][g]), reads=[DB["KsT"]], writes=[KsT[1]])
                    S.dma("sp", A("dma_start", out=KwT[0][:], in_=FT["KwT"][g]), reads=[DB["KwT"]], writes=[KwT[1]])
                    S.dma("sp", A("dma_start", out=Vs[0][:, :, 0:128],
                                  in_=TK["Vs"][:, :, g * 128:(g + 1) * 128].rearrange("t p c -> p t c")),
                          reads=[DB["Vs"]], writes=[Vs[1]])
                    S.dma("sp", A("dma_start", out=Vw[0][:, :, 0:128],
                                  in_=TK["Vw"][:, :, g * 128:(g + 1) * 128].rearrange("t p c -> p t c")),
                          reads=[DB["Vw"]], writes=[Vw[1]])
                    S.dma("sp", A("dma_start", out=Ng[0][:],
                                  in_=TK["Ng"][:, :, g * GQ * 128:(g + 1) * GQ * 128].rearrange("t p c -> p t c")),
                          reads=[DB["Ng"]], writes=[Ng[1]])
                    S.dma("sp", A("dma_start", out=cmpV[0][:], in_=c_cmpv[:, :]), writes=[cmpV[1]])
                    for kv in ("k", "v"):
                        pm, b_pm = mring.next()
                        S.op("pe", MM([dict(out=pm[:, 0:1], lhsT=W1[kv][:, j, :], rhs=peT[kv][:, j:j + 1]) for j in range(32)]),
                             reads=[b_k], writes=[b_pm])
                        S.op("act", A("copy", out=cb[0][:], in_=pm[:, 0:1]), reads=[b_pm], writes=[cb[1]])
                        pm, b_pm = mring.next()
                        S.op("pe", MM([dict(out=pm[:, 0:127], lhsT=W1[kv][:, j, :], rhs=XcT[kv][0][:, j:j + 2017:16])
                                       for j in range(32)]), reads=[b_k, XcT[kv][1]], writes=[b_pm])
                        S.op("act", A("activation", out=hT_[0][:, 0:127], in_=pm[:, 0:127], func=AF.Silu, bias=cb[0][:, 0:1], scale=1.0),
                             reads=[b_pm, cb[1]], writes=[hT_[1]])
                        pm, b_pm = mring.next()
                        S.op("pe", MM([dict(out=pm[0:127, 0:128], lhsT=hT_[0][:, 0:127], rhs=W2[kv][:])]),
                             reads=[hT_[1], b_k], writes=[b_pm])
                        if kv == "v":
                            S.op("act", A("copy", out=cmpV[0][:, 0:128], in_=pm[0:127, 0:128]), reads=[b_pm], writes=[cmpV[1]])
                        else:
                            S.op("act", A("copy", out=xs[0][:], in_=pm[0:127, 0:128]), reads=[b_pm], writes=[xs[1]])
                            S.op("act", A("activation", out=junk[0][:], in_=pm[0:127, 0:128], func=AF.Square, accum_out=ms[0][:, 0:1]),
                                 reads=[b_pm], writes=[junk[1], ms[1]])
                            rstd_ops(ms[0][:], rs1[0][:], ms[1], rs1[1], 1.0 / 128)
                            S.op("dve", A("scalar_tensor_tensor", out=xs[0][:], in0=xs[0][:], scalar=rs1[0][:, 0:1], in1=gk[0:127, :],
                                          op0=ALU.mult, op1=ALU.mult), reads=[xs[1], rs1[1], b_k], writes=[xs[1]])
                            rope_ops(xs[0][:].unsqueeze(1), kc[0][:].unsqueeze(1), xs[1], kc[1],
                                     cosc[:].unsqueeze(1), sinc[:].unsqueeze(1), b_k, tmps, 127, 1)
                            pm2, b_pm2 = mring.next()
                            ptv = pm2[:].bitcast(BF16)
                            S.op("pe", TR([dict(out=ptv[:, 0:127], in_=kc[0][:], identity=ident[0:127, 0:127])]),
                                 reads=[kc[1], b_const], writes=[b_pm2])
                            S.op("act", A("copy", out=kcmpT[0][:, 0:127], in_=ptv[:, 0:127]), reads=[b_pm2], writes=[kcmpT[1]])
                    for r in range(GQ):
                        h = g * GQ + r
                        QT, b_QT = QTs[r]
                        for c in range(4):
                            ps_s, b_s = sring.next()
                            S.op("pe", MM([dict(out=ps_s[0:127, :], lhsT=kcmpT[0][:, 0:127], rhs=QT[:, c * 512:(c + 1) * 512])]),
                                 reads=[kcmpT[1], b_QT], writes=[b_s])
                            pT, b_pT = pring.next()
                            S.op("act", A("activation", out=pT[0:127, :], in_=ps_s[0:127, :], func=AF.Exp, scale=SCALE),
                                 reads=[b_s], writes=[b_pT])
                            S.op("dve", A("tensor_tensor", out=pT[0:127, :], in0=pT[0:127, :], in1=cmaskT[:, c * 512:(c + 1) * 512],
                                          op=ALU.mult), reads=[b_pT, b_k], writes=[b_pT])
                            for q4 in range(4):
                                qt = c * 4 + q4
                                acc, b_acc = accs[qt % 4]
                                S.op("pe", MM([dict(out=acc[:, 0:161], lhsT=pT[0:127, q4 * 128:(q4 + 1) * 128], rhs=cmpV[0][:])]),
                                     reads=[b_pT, cmpV[1]], writes=[b_acc])
                                rs, b_rs = rsr.next()
                                cf, b_cf = cfr.next()
                                S.op("dve", A("tensor_scalar_max", out=rs[:], in0=acc[:, 128:129], scalar1=1e-30), reads=[b_acc], writes=[b_rs])
                                S.op("dve", A("reciprocal", out=rs[:], in_=rs[:]), reads=[b_rs], writes=[b_rs])
                                S.op("dve", A("tensor_tensor", out=cf[:], in0=rs[:], in1=Gl[:, qt, h:h + 1], op=ALU.mult),
                                     reads=[b_rs, b_k], writes=[b_cf])
                                S.op("dve", A("tensor_scalar_mul", out=nacc[:, qt, r, :], in0=acc[:, 0:128], scalar1=cf[:, 0:1]),
                                     reads=[b_acc, b_cf], writes=[nacc_b[qt][r]])
                                if r == 0:
                                    S.op("dve", A("tensor_scalar_mul", out=imp[:, qt, :], in0=acc[:, 129:161], scalar1=rs[:, 0:1]),
                                         reads=[b_acc, b_rs], writes=[imp_b[qt]])
                                else:
                                    S.op("dve", A("scalar_tensor_tensor", out=imp[:, qt, :], in0=acc[:, 129:161], scalar=rs[:, 0:1],
                                                  in1=imp[:, qt, :], op0=ALU.mult, op1=ALU.add),
                                         reads=[b_acc, b_rs, imp_b[qt]], writes=[imp_b[qt]])
                    for qt in range(NT):
                        sc, b_sc = scr.next()
                        sc2, b_sc2 = sc2r.next()
                        m8, b_m8 = m8r.next()
                        selb, b_sb = sbr.next()
                        S.op("dve", A("tensor_tensor", out=sc[:], in0=imp[:, qt, :], in1=M1[:, qt, :], op=ALU.mult),
                             reads=[imp_b[qt], b_k], writes=[b_sc])
                        S.op("dve", A("tensor_tensor", out=sc[:], in0=sc[:], in1=M2[:, qt, :], op=ALU.add),
                             reads=[b_sc, b_k], writes=[b_sc])
                        S.op("dve", A("max", out=m8[:, 0:8], in_=sc[:]), reads=[b_sc], writes=[b_m8])
                        S.op("dve", A("match_replace", out=sc2[:], in_to_replace=m8[:, 0:8], in_values=sc[:], imm_value=-3e9),
                             reads=[b_sc, b_m8], writes=[b_sc2])
                        S.op("dve", A("max", out=m8[:, 8:16], in_=sc2[:]), reads=[b_sc2, b_m8], writes=[b_m8])
                        S.op("dve", A("tensor_scalar", out=selb[:], in0=sc[:], scalar1=m8[:, 15:16], scalar2=1.0,
                                      op0=ALU.is_ge, op1=ALU.subtract), reads=[b_sc, b_m8], writes=[b_sb])
                        pm, b_pm = mring.next()
                        ptv = pm[:].bitcast(BF16)
                        S.op("pe", TR([dict(out=ptv[0:32, 0:128], in_=selb[:], identity=ident[:])]),
                             reads=[b_sb, b_const], writes=[b_pm])
                        S.op("act", A("copy", out=selT[:, qt * 128:(qt + 1) * 128], in_=ptv[0:32, 0:128]), reads=[b_pm], writes=[b_sel])
                    for r in range(GQ):
                        h = g * GQ + r
                        QT, b_QT = QTs[r]
                        for (mode, br, KT_, V_) in (("slc", 1, KsT, Vs), ("win", 2, KwT, Vw)):
                            def fin(qt, acc, b_acc, br=br, h=h, r=r):
                                rs, b_rs = rsr.next()
                                cf, b_cf = cfr.next()
                                S.op("dve", A("reciprocal", out=rs[:], in_=acc[:, 128:129]), reads=[b_acc], writes=[b_rs])
                                S.op("dve", A("tensor_tensor", out=cf[:], in0=rs[:], in1=Gl[:, qt, br * NH + h:br * NH + h + 1], op=ALU.mult),
                                     reads=[b_rs, b_k], writes=[b_cf])
                                S.op("dve", A("scalar_tensor_tensor", out=nacc[:, qt, r, :], in0=acc[:, 0:128], scalar=cf[:, 0:1],
                                              in1=nacc[:, qt, r, :], op0=ALU.mult, op1=ALU.add),
                                     reads=[b_acc, b_cf, nacc_b[qt][r]], writes=[nacc_b[qt][r]])
                            attention(mode, QT, b_QT, KT_[0], KT_[1], (lambda kt, V_=V_: V_[0][:, kt, :]), V_[1], 129, rings, fin)
                    for qt in range(NT):
                        S.op("pool", A("tensor_tensor", out=ybuf[:, qt, :], in0=nacc[:, qt, :, :].rearrange("p r d -> p (r d)"),
                                       in1=Ng[0][:, qt, :], op=ALU.mult),
                             reads=[nacc_b[qt][r] for r in range(GQ)] + [Ng[1]], writes=[b_y])
                    c0 = RH * 128 + g * GQ * 128
                    S.dma("sp", A("dma_start", out=TK["Y0"][:, :, c0:c0 + GQ * 128].rearrange("t p c -> p t c"), in_=ybuf[:]),
                          reads=[b_y], writes=[DB["Y0"]])
            S.barrier()

        def phase_diff():
            with ExitStack() as ps_:
                sb = lambda n, s, d: ps_.enter_context(nc.sbuf_tensor(uname(n), list(s), d))
                pp = lambda n, s, d: ps_.enter_context(nc.psum_tensor(uname(n), list(s), d))
                b_k = Buf()
                tri = sb("d_tri", [128, 128], BF16)
                S.dma("sp", A("dma_start", out=tri[:], in_=c_tri[:, :]), writes=[b_k])
                lam_t = sb("d_lam", [128, 4, 128], F32)
                S.dma("sp", A("dma_start", out=lam_t[:], in_=lam_in[:, :, :]), writes=[b_k])
                prod = sb("d_prod", [128, 2, 128], F32)
                sums = sb("d_sums", [128, 2], F32)
                nlam = sb("d_nlam", [128, 1], F32)
                b_l = Buf()
                S.op("dve", A("tensor_tensor", out=prod[:, 0, :], in0=lam_t[:, 0, :], in1=lam_t[:, 1, :], op=ALU.mult), reads=[b_k], writes=[b_l])
                S.op("dve", A("tensor_tensor", out=prod[:, 1, :], in0=lam_t[:, 2, :], in1=lam_t[:, 3, :], op=ALU.mult), reads=[b_k, b_l], writes=[b_l])
                S.op("dve", A("tensor_reduce", out=sums[:], in_=prod[:], axis=AX.X, op=ALU.add), reads=[b_l], writes=[b_l])
                S.op("act", A("activation", out=sums[:], in_=sums[:], func=AF.Exp), reads=[b_l], writes=[b_l])
                S.op("dve", A("tensor_tensor", out=nlam[:], in0=sums[:, 1:2], in1=sums[:, 0:1], op=ALU.subtract), reads=[b_l], writes=[b_l])
                S.op("dve", A("tensor_scalar_add", out=nlam[:], in0=nlam[:], scalar1=-LAMBDA_INIT), reads=[b_l], writes=[b_l])
                NS = 2
                slots = []
                for i in range(NS):
                    sl = dict(
                        Q=[(sb("d_Q%d_%d" % (i, c), [128, S_LEN], BF16), Buf()) for c in range(2)],
                        K=[(sb("d_K%d_%d" % (i, c), [128, S_LEN], BF16), Buf()) for c in range(2)],
                        V=(sb("d_V%d" % i, [128, NT, 257], BF16), Buf()),
                        G=(sb("d_G%d" % i, [128, NT, 256], BF16), Buf()),
                        Y=(sb("d_Y%d" % i, [128, NT, 256], BF16), Buf()),
                    )
                    S.op("dve", A("memset", ap=sl["V"][0][:, :, 256:257], constant=1.0), writes=[sl["V"][1]])
                    slots.append(sl)
                o1 = sb("d_o1", [128, NT, 256], F32)
                o1_b = [Buf() for _ in range(NT)]
                otr = Ring([sb("d_ot%d" % i, [128, 256], F32) for i in range(2)])
                junk = (sb("d_junk", [128, 256], BF16), Buf())
                sring = Ring([pp("d_ps%d" % i, [128, 512], F32) for i in range(2)])
                accs = [(pp("d_acc%d" % i, [128, 512], F32), Buf()) for i in range(4)]
                pring = Ring([sb("d_pT%d" % i, [128, 512], BF16) for i in range(3)])
                rsr = Ring([sb("d_rs%d" % i, [128, 1], F32) for i in range(4)])
                cfr = Ring([sb("d_cf%d" % i, [128, 1], F32) for i in range(4)])
                ssr = Ring([sb("d_ss%d" % i, [128, 1], F32) for i in range(4)])
                r2r = Ring([sb("d_r2%d" % i, [128, 1], F32) for i in range(4)])
                rings = (sring, pring, accs, None, None, None, tri, None, b_k)

                def load(h):
                    sl = slots[h % NS]
                    for c in range(2):
                        S.dma("sp", A("dma_start", out=sl["Q"][c][0][:], in_=FT["Q1T"][2 * h + c]), reads=[DB["Q1T"]], writes=[sl["Q"][c][1]])
                        S.dma("sp", A("dma_start", out=sl["K"][c][0][:], in_=FT["K1T"][2 * h + c]), reads=[DB["K1T"]], writes=[sl["K"][c][1]])
                    S.dma("sp", A("dma_start", out=sl["V"][0][:, :, 0:256],
                                  in_=TK["V1"][:, :, h * 256:(h + 1) * 256].rearrange("t p c -> p t c")),
                          reads=[DB["V1"]], writes=[sl["V"][1]])
                    S.dma("sp", A("dma_start", out=sl["G"][0][:],
                                  in_=TK["G1"][:, :, h * 256:(h + 1) * 256].rearrange("t p c -> p t c")),
                          reads=[DB["G1"]], writes=[sl["G"][1]])

                load(0)
                for h in range(DH):
                    if h + 1 < DH:
                        load(h + 1)
                    sl = slots[h % NS]

                    def fin0(qt, acc, b_acc):
                        rs, b_rs = rsr.next()
                        S.op("dve", A("reciprocal", out=rs[:], in_=acc[:, 256:257]), reads=[b_acc], writes=[b_rs])
                        S.op("dve", A("tensor_scalar_mul", out=o1[:, qt, :], in0=acc[:, 0:256], scalar1=rs[:, 0:1]),
                             reads=[b_acc, b_rs], writes=[o1_b[qt]])

                    def fin1(qt, acc, b_acc, sl=sl):
                        rs, b_rs = rsr.next()
                        cf, b_cf = cfr.next()
                        ot, b_ot = otr.next()
                        ss, b_ss = ssr.next()
                        r2, b_r2 = r2r.next()
                        S.op("dve", A("reciprocal", out=rs[:], in_=acc[:, 256:257]), reads=[b_acc], writes=[b_rs])
                        S.op("dve", A("tensor_tensor", out=cf[:], in0=rs[:], in1=nlam[:], op=ALU.mult), reads=[b_rs, b_l], writes=[b_cf])
                        S.op("dve", A("scalar_tensor_tensor", out=ot[:], in0=acc[:, 0:256], scalar=cf[:, 0:1], in1=o1[:, qt, :],
                                      op0=ALU.mult, op1=ALU.add), reads=[b_acc, b_cf, o1_b[qt]], writes=[b_ot])
                        S.op("act", A("activation", out=junk[0][:], in_=ot[:], func=AF.Square, accum_out=ss[:, 0:1]),
                             reads=[b_ot], writes=[junk[1], b_ss])
                        rstd_ops(ss[:], r2[:], b_ss, b_r2, 1.0 / 256)
                        S.op("dve", A("tensor_scalar_mul", out=r2[:], in0=r2[:], scalar1=float(1.0 - LAMBDA_INIT)), reads=[b_r2], writes=[b_r2])
                        S.op("dve", A("scalar_tensor_tensor", out=sl["Y"][0][:, qt, :], in0=ot[:], scalar=r2[:, 0:1], in1=sl["G"][0][:, qt, :],
                                      op0=ALU.mult, op1=ALU.mult), reads=[b_ot, b_r2, sl["G"][1]], writes=[sl["Y"][1]])

                    for c, fin in ((0, fin0), (1, fin1)):
                        attention("causal", sl["Q"][c][0], sl["Q"][c][1], sl["K"][c][0], sl["K"][c][1],
                                  (lambda kt, sl=sl: sl["V"][0][:, kt, :]), sl["V"][1], 257, rings, fin)
                    S.dma("sp", A("dma_start", out=TK["Y1"][:, :, h * 256:(h + 1) * 256].rearrange("t p c -> p t c"), in_=sl["Y"][0][:]),
                          reads=[sl["Y"][1]], writes=[DB["Y1"]])
            S.barrier()

        def phase_outproj(yname, w_dram, res_tiles, res_buf, dst_tiles, dst_buf):
            with ExitStack() as ps_:
                sb = lambda n, s, d: ps_.enter_context(nc.sbuf_tensor(uname(n), list(s), d))
                pp = lambda n, s, d: ps_.enter_context(nc.psum_tensor(uname(n), list(s), d))
                w = sb("c_w", [128, 16, D], BF16)
                w_b = [Buf() for _ in range(4)]
                for cbk in range(4):
                    for hf in range(2):
                        S.dma("pool", A("dma_start", out=w[:, hf * 8:(hf + 1) * 8, cbk * 512:(cbk + 1) * 512],
                                        in_=w_dram[hf * 1024:(hf + 1) * 1024, cbk * 512:(cbk + 1) * 512].rearrange("(k p) n -> p k n", p=128)),
                              writes=[w_b[cbk]])
                yr = Ring([sb("c_y%d" % i, [128, D], BF16) for i in range(2)])
                yTr = Ring([sb("c_yT%d" % i, [128, 16, 128], BF16) for i in range(2)])
                xrr = Ring([sb("c_xr%d" % i, [128, D], F32) for i in range(2)])
                xor_ = Ring([sb("c_xo%d" % i, [128, D], F32) for i in range(2)])
                ptr = Ring([pp("c_pt%d" % i, [128, 8, 128], BF16) for i in range(2)])
                psr = Ring([pp("c_ps%d" % i, [128, 512], F32) for i in range(3)])

                def stage1(t):
                    yt, b_yt = yr.next()
                    S.dma("sp", A("dma_start", out=yt[:], in_=TK[yname][t]), reads=[DB[yname]], writes=[b_yt])
                    xr, b_xr = xrr.next()
                    S.dma("sp", A("dma_start", out=xr[:], in_=res_tiles[t]), reads=[res_buf] if res_buf is not None else [], writes=[b_xr])
                    yT, b_yT = yTr.next()
                    for half in range(2):
                        pt, b_pt = ptr.next()
                        S.op("pe", TR([dict(out=pt[:, jj, :], in_=yt[:, (half * 8 + jj) * 128:(half * 8 + jj + 1) * 128], identity=ident[:])
                                       for jj in range(8)]), reads=[b_yt, b_const], writes=[b_pt])
                        if half == 0:
                            S.op("act", A("copy", out=yT[:, 0:8, :], in_=pt[:]), reads=[b_pt], writes=[b_yT])
                        else:
                            S.op("dve", A("tensor_copy", out=yT[:, 8:16, :], in_=pt[:]), reads=[b_pt], writes=[b_yT])
                    return yT, b_yT, xr, b_xr

                def stage2(t, yT, b_yT, xr, b_xr):
                    xo, b_xo = xor_.next()
                    for cbk in range(4):
                        ps, b_ps = psr.next()
                        S.op("pe", MM([dict(out=ps[:], lhsT=yT[:, k, :], rhs=w[:, k, cbk * 512:(cbk + 1) * 512]) for k in range(16)]),
                             reads=[b_yT, w_b[cbk]], writes=[b_ps])
                        S.op("dve", A("tensor_tensor", out=xo[:, cbk * 512:(cbk + 1) * 512], in0=ps[:], in1=xr[:, cbk * 512:(cbk + 1) * 512],
                                      op=ALU.add), reads=[b_ps, b_xr], writes=[b_xo])
                    S.dma("sp", A("dma_start", out=dst_tiles[t], in_=xo[:]), reads=[b_xo], writes=[dst_buf])

                cur = stage1(0)
                for t in range(NT):
                    nxt = stage1(t + 1) if t + 1 < NT else None
                    stage2(t, *cur)
                    cur = nxt
            S.barrier()

        sec0 = [
            dict(kind="rope", ncols=RH * 128, ft="QrT"),
            dict(kind="rope_k", ncols=RH * 128, ft="KrT", tk="Kd"),
            dict(kind="plain", ncols=RH * 128, tk="Vr"),
            dict(kind="silu", ncols=RH * 128, tk="Rg"),
            dict(kind="normrope_q", ncols=NH * 128, ft="QnT"),
            dict(kind="plainT", ncols=NG * 128, ft="KcT"),
            dict(kind="plainT", ncols=NG * 128, ft="VcT"),
            dict(kind="normrope_k", ncols=NG * 128, ft="KsT"),
            dict(kind="plain", ncols=NG * 128, tk="Vs"),
            dict(kind="normrope_k", ncols=NG * 128, ft="KwT"),
            dict(kind="plain", ncols=NG * 128, tk="Vw"),
            dict(kind="silu", ncols=NH * 128, tk="Ng"),
            dict(kind="sigmoid", ncols=3 * NH, tk="Gl"),
        ]
        sec1 = [
            dict(kind="normrope_q", ncols=2 * DH * 128, ft="Q1T"),
            dict(kind="normrope_k", ncols=2 * DH * 128, ft="K1T"),
            dict(kind="plain", ncols=DH * 256, tk="V1"),
            dict(kind="silu", ncols=DH * 256, tk="G1"),
        ]

        def run():
            order = ["N0A0", "ret", "nsa", "C0", "N1A1", "B1", "C1"]
            i0 = order.index(start_at) if start_at else 0
            todo = order[i0:]
            if "N0A0" in todo:
                with ExitStack() as hs:
                    hT = hs.enter_context(nc.sbuf_tensor("hT0_sb", [128, 16, S_LEN], BF16))
                    hT_b = [Buf() for _ in range(NT)]
                    phase_norm(x_tiles, None, g_norm0, hT, hT_b)
                    if stop_after == "N0":
                        return
                    phase_inproj(w_in0, NC0, sec0, hT, hT_b, g_q0, g_k0, c_kdec)
                if stop_after == "A0":
                    return
            if "ret" in todo:
                phase_ret()
                if stop_after == "ret":
                    return
            if "nsa" in todo:
                phase_nsa()
                if stop_after == "nsa":
                    return
            if "C0" in todo:
                phase_outproj("Y0", w_out0, x_tiles, None, TK["X1"], DB["X1"])
                if stop_after == "C0":
                    return
            if "N1A1" in todo:
                with ExitStack() as hs:
                    hT = hs.enter_context(nc.sbuf_tensor("hT1_sb", [128, 16, S_LEN], BF16))
                    hT_b = [Buf() for _ in range(NT)]
                    phase_norm(TK["X1"], DB["X1"], g_norm1, hT, hT_b)
                    phase_inproj(w_in1, NC1, sec1, hT, hT_b, g_q1, g_k1, None)
                if stop_after == "A1":
                    return
            if "B1" in todo:
                phase_diff()
                if stop_after == "B1":
                    return
            phase_outproj("Y1", w_out1, TK["X1"], DB["X1"], out_t, out_buf)

        run()
        S.barrier()
        S.emit()
    return nc


def make_in_maps(inputs, consts):
    f32 = lambda a: np.ascontiguousarray(np.asarray(a, dtype=np.float32))
    rep = lambda v: np.ascontiguousarray(np.broadcast_to(f32(v)[None, :], (128, f32(v).shape[0])))
    shared = dict(
        w_in0=f32(inputs["l0_w_in"]), w_out0=f32(inputs["l0_w_out"]),
        w_in1=f32(inputs["l1_w_in"]), w_out1=f32(inputs["l1_w_out"]),
        g_norm0=rep(inputs["l0_norm_g"]), g_norm1=rep(inputs["l1_norm_g"]),
        g_q0=rep(inputs["l0_nsa_q_norm_g"]), g_k0=rep(inputs["l0_nsa_k_norm_g"]),
        g_q1=rep(inputs["l1_q_norm_g"]), g_k1=rep(inputs["l1_k_norm_g"]),
        lam_in=np.ascontiguousarray(np.broadcast_to(
            np.stack([f32(inputs["l1_lambda_q1"]), f32(inputs["l1_lambda_k1"]),
                      f32(inputs["l1_lambda_q2"]), f32(inputs["l1_lambda_k2"])])[None], (128, 4, 128))),
        w1k=f32(inputs["l0_cmp_w1_k"]), w1v=f32(inputs["l0_cmp_w1_v"]),
        w2k=f32(inputs["l0_cmp_w2_k"]), w2v=f32(inputs["l0_cmp_w2_v"]),
        pek=np.ascontiguousarray(f32(inputs["l0_cmp_pe_k"]).T), pev=np.ascontiguousarray(f32(inputs["l0_cmp_pe_v"]).T),
    )
    shared.update(consts)
    x = f32(inputs["x"])
    maps = []
    for c in range(8):
        m = dict(shared)
        m["x"] = np.ascontiguousarray(x[c % 4])
        maps.append(m)
    return maps


def kernel(**inputs):
    consts, cdec = make_consts(list(range(8)))
    nc = build(cdec=cdec)
    maps = make_in_maps(inputs, consts)
    res = run_bass_kernel_spmd(nc, maps, core_ids=list(range(8)))
    out = np.stack([np.asarray(res.results[b]["out"]).reshape(S_LEN, D) for b in range(4)]).astype(np.float32)
    return out
```

```python
import math
from contextlib import ExitStack
import numpy as np
import ml_dtypes
import concourse.bass as bass
import concourse.mybir as mybir
from concourse.bass_utils import run_bass_kernel_spmd

F32 = mybir.dt.float32
BF16 = mybir.dt.bfloat16
AF = mybir.ActivationFunctionType
ALU = mybir.AluOpType
AX = mybir.AxisListType
NPBF = ml_dtypes.bfloat16

S_LEN = 2048
D = 2048
NT = 16
HD = 128
EPS = 1e-6
SCALE = HD ** -0.5
NEG = -30000.0
LAMBDA_INIT = 0.8 - 0.6 * math.exp(-0.3 * 1)
NDMA = 48


class Buf:
    __slots__ = ("name", "w", "r")

    def __init__(self, name=""):
        self.name = name
        self.w = None
        self.r = []


class Sync:
    ENGS = ("pe", "act", "dve", "pool", "sp")

    def __init__(self, nc, es):
        self.nc = nc
        self.sem = {k: es.enter_context(nc.semaphore("s_" + k)) for k in self.ENGS}
        self.cnt = {k: 0 for k in self.ENGS}
        self.seen = {k: {} for k in self.ENGS}
        self.prog = {k: [] for k in self.ENGS}
        self.dq = {"sp": ("d", 28), "pool": ("g", 12)}
        self.dma_val = {}
        self.dma_next = {q: 0 for q in self.dq}
        for q, (pfx, n) in self.dq.items():
            for i in range(n):
                k = "%s%d" % (pfx, i)
                self.sem[k] = es.enter_context(nc.semaphore(k))
                self.dma_val[k] = 0
        self.ncc = 0
        for i in range(2):
            self.sem["cc%d" % i] = es.enter_context(nc.semaphore("cc%d" % i))
            self.dma_val["cc%d" % i] = 0

    def _deps(self, e, reads, writes, extra=()):
        need = {}

        def add(tok):
            if tok is None:
                return
            k, v = tok
            if need.get(k, 0) < v:
                need[k] = v
        for b in reads:
            add(b.w)
        for b in writes:
            add(b.w)
            for t in b.r:
                add(t)
        for t in extra:
            add(t)
        waits = []
        seen = self.seen[e]
        for k, v in need.items():
            if k == e and e == "pe":
                continue
            if seen.get(k, 0) >= v:
                continue
            seen[k] = v
            waits.append((k, v))
        return waits

    def _mark(self, tok, reads, writes):
        for b in reads:
            b.r.append(tok)
            if len(b.r) > 64:
                mx = {}
                for (k, v) in b.r:
                    if mx.get(k, 0) < v:
                        mx[k] = v
                b.r = list(mx.items())
        for b in writes:
            b.w = tok
            b.r = []

    def op(self, e, fn, reads=(), writes=()):
        waits = self._deps(e, reads, writes)
        self.cnt[e] += 1
        tok = (e, self.cnt[e])
        self.prog[e].append((waits, fn, (e, 1)))
        self._mark(tok, reads, writes)
        return tok

    def dma(self, q, fn, reads=(), writes=()):
        pfx, n = self.dq[q]
        i = self.dma_next[q]
        self.dma_next[q] = (i + 1) % n
        k = "%s%d" % (pfx, i)
        extra = []
        if self.dma_val[k] > 0:
            extra.append((k, self.dma_val[k]))
        waits = self._deps(q, reads, writes, extra)
        self.dma_val[k] += 16
        tok = (k, self.dma_val[k])
        self.prog[q].append((waits, fn, (k, 16)))
        self._mark(tok, reads, writes)
        return tok

    def collective(self, fn, inc, reads=(), writes=()):
        k = "cc%d" % self.ncc
        self.ncc += 1
        waits = self._deps("pool", reads, writes)
        self.dma_val[k] += inc
        tok = (k, self.dma_val[k])
        self.prog["pool"].append((waits, fn, (k, inc)))
        self._mark(tok, reads, writes)
        return tok

    def barrier(self):
        toks = [(k, self.cnt[k]) for k in self.ENGS if self.cnt[k] > 0]
        toks += [(k, v) for k, v in self.dma_val.items() if v > 0]
        for e in self.ENGS:
            waits = []
            for (k, v) in toks:
                if k == e and e == "pe":
                    continue
                if self.seen[e].get(k, 0) >= v:
                    continue
                self.seen[e][k] = v
                waits.append((k, v))
            self.prog[e].append((waits, None, None))

    def emit(self):
        nc = self.nc
        sem = self.sem

        def replay(key, eng):
            for waits, fn, inc in self.prog[key]:
                for (k, v) in waits:
                    eng.wait_ge(sem[k], v)
                if fn is None:
                    continue
                ins = fn(eng)
                if inc is not None:
                    ins.then_inc(sem[inc[0]], inc[1])

        with nc.Block() as block:
            @block.tensor
            def _(eng):
                replay("pe", eng)

            @block.scalar
            def _(eng):
                replay("act", eng)

            @block.vector
            def _(eng):
                replay("dve", eng)

            @block.gpsimd
            def _(eng):
                replay("pool", eng)

            @block.sync
            def _(eng):
                replay("sp", eng)


class Ring:
    def __init__(self, tensors):
        self.items = [(t, Buf()) for t in tensors]
        self.i = 0

    def next(self):
        it = self.items[self.i % len(self.items)]
        self.i += 1
        return it


def _rope_np(pos):
    inv = (1.0 / (np.float32(10000.0) ** (np.arange(0, HD, 2, dtype=np.float32) / np.float32(HD)))).astype(np.float32)
    ang = pos.astype(np.float32)[:, None] * inv[None, :]
    return np.cos(ang).astype(np.float32), np.sin(ang).astype(np.float32)


def make_consts(ret_heads):
    c = {}
    c["ident"] = np.eye(128, dtype=np.float32).astype(NPBF)
    cos, sin = _rope_np(np.arange(S_LEN))
    c["cosT"] = np.ascontiguousarray(cos.reshape(NT, 128, 64).transpose(1, 0, 2))
    c["sinT"] = np.ascontiguousarray(sin.reshape(NT, 128, 64).transpose(1, 0, 2))
    cc, sc = _rope_np(np.arange(127) * 16 + 31)
    c["cosc"] = cc
    c["sinc"] = sc
    RH = len(ret_heads)
    j = np.arange(128, dtype=np.float64)
    DT = np.zeros((128, RH, 128), np.float32)
    qdec = np.zeros((128, RH, 128), np.float32)
    kdec = np.zeros((128, RH), np.float32)
    cdec = []
    for li, h in enumerate(ret_heads):
        lg = np.log1p(-np.exp2(-5.0 - h))
        diff = j[None, :] - j[:, None]
        DT[:, li, :] = np.where(diff >= 0, np.exp(lg * np.maximum(diff, 0.0)), 0.0) * SCALE
        qdec[:, li, :] = np.exp(lg * (j + 1.0))[None, :]
        kdec[:, li] = np.exp(lg * (127.0 - j)) * SCALE
        cdec.append(float(np.exp(lg * 128.0)))
    c["DT"] = DT
    c["qdec"] = qdec
    c["kdec"] = kdec
    c["cdec"] = np.ascontiguousarray(np.broadcast_to(np.asarray(cdec, np.float32)[None, :], (128, RH)))
    n = np.arange(127)
    q = np.arange(S_LEN)
    c["cmaskT"] = ((n[:, None] * 16 + 31) <= q[None, :]).astype(np.float32).astype(NPBF)
    c_start = n * 16
    s_start = np.arange(32) * 64
    ov = ((c_start[:, None] <= s_start[None, :] + 63) & (c_start[:, None] + 31 >= s_start[None, :])).astype(np.float32)
    cv = np.zeros((127, 161), np.float32)
    cv[:, 128] = 1.0
    cv[:, 129:161] = ov
    c["cmpv_init"] = cv.astype(NPBF)
    E = np.zeros((32, NT, 128), np.float32)
    for kt in range(NT):
        for kl in range(128):
            E[2 * kt + kl // 64, kt, kl] = -NEG
    c["E"] = E.astype(NPBF)
    kk = np.arange(128)[:, None]
    qq = np.arange(128)[None, :]
    c["tri"] = np.where(kk > qq, NEG, 0.0).astype(np.float32).astype(NPBF)
    c["anti"] = np.where(qq >= kk, NEG, 0.0).astype(np.float32).astype(NPBF)
    M1 = np.zeros((128, NT, 32), np.float32)
    M2 = np.zeros((128, NT, 32), np.float32)
    for t in range(NT):
        for p in range(128):
            blk = 2 * t + p // 64
            for m in range(32):
                back = blk - m
                forced = (m == 0) or (0 <= back < 2)
                causal = back >= 0
                if forced:
                    M2[p, t, m] = 3e9 if m == 0 else (2e9 if back == 0 else 1e9)
                elif causal:
                    M1[p, t, m] = 1.0
                else:
                    M2[p, t, m] = -1e9 - 1e7 * m
    c["M1"] = M1
    c["M2"] = M2
    return c, cdec


def A(fn, **kw):
    return lambda e: getattr(e, fn)(**kw)


def MM(lst):
    def f(e):
        n = len(lst)
        ins = None
        for i, d in enumerate(lst):
            ins = e.matmul(d["out"], lhsT=d["lhsT"], rhs=d["rhs"], start=(i == 0), stop=(i == n - 1))
        return ins
    return f


def MMRAW(lst):
    def f(e):
        ins = None
        for d in lst:
            ins = e.matmul(d["out"], lhsT=d["lhsT"], rhs=d["rhs"], start=d["start"], stop=d["stop"])
        return ins
    return f


def TR(lst):
    def f(e):
        ins = None
        for d in lst:
            ins = e.transpose(out=d["out"], in_=d["in_"], identity=d["identity"])
        return ins
    return f


def build(RH=8, NH=8, NG=2, DH=8, split=False, debug=(), stop_after=None, start_at=None, feed=(), cc_inc=1, ncores=8):
    nc = bass.Bass("TRN2", target_bir_lowering=False)
    GQ = NH // NG
    NC0 = 4 * RH * 128 + 2 * NH * 128 + 6 * NG * 128 + 3 * NH
    NC1 = 8 * DH * 128

    def din(name, shape, dt=F32):
        return nc.dram_tensor(name, list(shape), dt, kind="ExternalInput").ap()

    x_in = din("x", [S_LEN, D])
    w_in0 = din("w_in0", [D, NC0])
    w_out0 = din("w_out0", [D, D])
    w_in1 = din("w_in1", [D, NC1])
    w_out1 = din("w_out1", [D, D // 2 if split else D])
    g_norm0 = din("g_norm0", [128, D])
    g_norm1 = din("g_norm1", [128, D])
    g_q0 = din("g_q0", [128, 128])
    g_k0 = din("g_k0", [128, 128])
    g_q1 = din("g_q1", [128, 128])
    g_k1 = din("g_k1", [128, 128])
    lam_in = din("lam_in", [128, 4, 128])
    w1_in = {"k": din("w1k", [4096, 128]), "v": din("w1v", [4096, 128])}
    w2_in = {"k": din("w2k", [128, 128]), "v": din("w2v", [128, 128])}
    pe_in = {"k": din("pek", [128, 32]), "v": din("pev", [128, 32])}
    c_ident = din("ident", [128, 128], BF16)
    c_cosT = din("cosT", [128, NT, 64])
    c_sinT = din("sinT", [128, NT, 64])
    c_cosc = din("cosc", [127, 64])
    c_sinc = din("sinc", [127, 64])
    c_DT = din("DT", [128, RH, 128])
    c_qdec = din("qdec", [128, RH, 128])
    c_kdec = din("kdec", [128, RH])
    c_cdec = din("cdec", [128, RH])
    c_cmaskT = din("cmaskT", [127, S_LEN], BF16)
    c_cmpv = din("cmpv_init", [127, 161], BF16)
    c_E = din("E", [32, NT, 128], BF16)
    c_tri = din("tri", [128, 128], BF16)
    c_anti = din("anti", [128, 128], BF16)
    c_M1 = din("M1", [128, NT, 32])
    c_M2 = din("M2", [128, NT, 32])
    yidx_in = nc.dram_tensor("yidx", [128, 2 * NT], mybir.dt.int32, kind="ExternalInput").ap() if split else None

    OW = D // 2 if split else D
    YW = D // 2 if split else D
    out_t = nc.dram_tensor("out", [NT, 128, OW], F32, kind="ExternalOutput").ap()

    def scratch(name, shape, dt=BF16):
        kind = "ExternalOutput" if name in debug else ("ExternalInput" if name in feed else "Internal")
        return nc.dram_tensor(name, list(shape), dt, kind=kind).ap()

    FT = {}
    TK = {}
    for nm, nh in (("QrT", RH), ("KrT", RH), ("QnT", NH), ("KcT", NG), ("VcT", NG), ("KsT", NG), ("KwT", NG),
                   ("Q1T", 2 * DH), ("K1T", 2 * DH)):
        FT[nm] = scratch(nm, [nh, 128, S_LEN])
    for nm, ncol in (("Kd", RH * 128), ("Vr", RH * 128), ("Rg", RH * 128), ("Vs", NG * 128), ("Vw", NG * 128),
                     ("Ng", NH * 128), ("V1", DH * 256), ("G1", DH * 256)):
        TK[nm] = scratch(nm, [NT, 128, ncol])
    TK["Gl"] = scratch("Gl", [NT, 128, 3 * NH], F32)
    YG = {}
    for nm in ("Y0", "Y1"):
        yl = scratch(nm, [S_LEN, YW])
        TK[nm] = yl.rearrange("(t p) c -> t p c", p=128)
        if split:
            YG[nm] = (yl, scratch(nm + "g", [ncores * S_LEN, YW]))
    TK["X1"] = scratch("X1", [NT, 128, D], F32)
    DB = {k: Buf(k) for k in list(FT) + list(TK) + ["Y0g", "Y1g"]}
    out_buf = Buf("out")
    x_tiles = x_in.rearrange("(t p) d -> t p d", p=128)

    with ExitStack() as es:
        S = Sync(nc, es)

        uid = [0]

        def uname(n):
            uid[0] += 1
            return "%s_u%d" % (n, uid[0])

        def gsb(name, shape, dt):
            return es.enter_context(nc.sbuf_tensor(uname(name), list(shape), dt))
        ident = gsb("ident", [128, 128], BF16)
        cosT = gsb("cosT", [128, NT, 64], F32)
        sinT = gsb("sinT", [128, NT, 64], F32)
        b_const = Buf("const")
        S.dma("sp", A("dma_start", out=ident[:], in_=c_ident[:, :]), writes=[b_const])
        S.dma("sp", A("dma_start", out=cosT[:], in_=c_cosT[:, :, :]), writes=[b_const])
        S.dma("sp", A("dma_start", out=sinT[:], in_=c_sinT[:, :, :]), writes=[b_const])

        def rstd_ops(ss_ap, out_ap, b_ss, b_out, inv_n):
            S.op("dve", A("tensor_scalar", out=out_ap, in0=ss_ap, scalar1=inv_n, scalar2=EPS,
                          op0=ALU.mult, op1=ALU.add), reads=[b_ss], writes=[b_out])
            S.op("act", A("sqrt", out=out_ap, in_=out_ap), reads=[b_out], writes=[b_out])
            S.op("dve", A("reciprocal", out=out_ap, in_=out_ap), reads=[b_out], writes=[b_out])

        def rope_ops(xv, rv, b_xs, b_rb, cb, sbb, b_tab, tmps, P, n):
            (t1, b1), (t2, b2), (t3, b3), (t4, b4) = tmps
            v = lambda tt: tt[0:P, 0:n * 64].rearrange("p (h d) -> p h d", d=64)
            S.op("dve", A("tensor_tensor", out=v(t1), in0=xv[:, :, 0:64], in1=cb, op=ALU.mult),
                 reads=[b_xs, b_tab], writes=[b1])
            S.op("dve", A("tensor_tensor", out=v(t2), in0=xv[:, :, 64:128], in1=sbb, op=ALU.mult),
                 reads=[b_xs, b_tab], writes=[b2])
            S.op("dve", A("tensor_tensor", out=rv[:, :, 0:64], in0=v(t1), in1=v(t2), op=ALU.subtract),
                 reads=[b1, b2], writes=[b_rb])
            S.op("pool", A("tensor_tensor", out=v(t3), in0=xv[:, :, 0:64], in1=sbb, op=ALU.mult),
                 reads=[b_xs, b_tab], writes=[b3])
            S.op("pool", A("tensor_tensor", out=v(t4), in0=xv[:, :, 64:128], in1=cb, op=ALU.mult),
                 reads=[b_xs, b_tab], writes=[b4])
            S.op("pool", A("tensor_tensor", out=rv[:, :, 64:128], in0=v(t3), in1=v(t4), op=ALU.add),
                 reads=[b3, b4], writes=[b_rb])

        def phase_norm(src_tiles, src_buf, g_dram, hT, hT_b):
            with ExitStack() as ps_:
                sb = lambda n, s, d: ps_.enter_context(nc.sbuf_tensor(uname(n), list(s), d))
                pp = lambda n, s, d: ps_.enter_context(nc.psum_tensor(uname(n), list(s), d))
                gt = sb("n_g", [128, D], F32)
                b_g = Buf()
                S.dma("sp", A("dma_start", out=gt[:], in_=g_dram[:, :]), writes=[b_g])
                xr = Ring([sb("n_x%d" % i, [128, D], F32) for i in range(2)])
                junk = sb("n_junk", [128, D], BF16)
                b_junk = Buf()
                ssr = Ring([sb("n_ss%d" % i, [128, 1], F32) for i in range(2)])
                rsr = Ring([sb("n_rs%d" % i, [128, 1], F32) for i in range(2)])
                hbr = Ring([sb("n_hb%d" % i, [128, D], BF16) for i in range(2)])
                ptr = Ring([pp("n_pt%d" % i, [128, 8, 128], BF16) for i in range(2)])
                for t in range(NT):
                    xt, b_x = xr.next()
                    S.dma("sp", A("dma_start", out=xt[:], in_=src_tiles[t]),
                          reads=[src_buf] if src_buf is not None else [], writes=[b_x])
                    ss, b_ss = ssr.next()
                    rs, b_rs = rsr.next()
                    S.op("act", A("activation", out=junk[:], in_=xt[:], func=AF.Square, accum_out=ss[:, 0:1]),
                         reads=[b_x], writes=[b_junk, b_ss])
                    rstd_ops(ss[:], rs[:], b_ss, b_rs, 1.0 / D)
                    hb, b_hb = hbr.next()
                    S.op("dve", A("scalar_tensor_tensor", out=hb[:], in0=xt[:], scalar=rs[:, 0:1], in1=gt[:],
                                  op0=ALU.mult, op1=ALU.mult), reads=[b_x, b_rs, b_g], writes=[b_hb])
                    for half in range(2):
                        pt, b_pt = ptr.next()
                        S.op("pe", TR([dict(out=pt[:, jj, :], in_=hb[:, (half * 8 + jj) * 128:(half * 8 + jj + 1) * 128],
                                            identity=ident[:]) for jj in range(8)]),
                             reads=[b_hb, b_const], writes=[b_pt])
                        dst = hT[:, half * 8:(half + 1) * 8, t * 128:(t + 1) * 128]
                        if half == 0:
                            S.op("act", A("copy", out=dst, in_=pt[:]), reads=[b_pt], writes=[hT_b[t]])
                        else:
                            S.op("dve", A("tensor_copy", out=dst, in_=pt[:]), reads=[b_pt], writes=[hT_b[t]])
            S.barrier()

        def phase_inproj(w_dram, ncols, sections, hT, hT_b, gq_dram, gk_dram, kdec_dram):
            segs = []
            col = 0
            for si, sec in enumerate(sections):
                w_ = sec["ncols"]
                if w_ % 128 == 0:
                    for hh in range(w_ // 128):
                        segs.append((col + hh * 128, 128, si, hh))
                else:
                    segs.append((col, w_, si, 0))
                col += w_
            assert col == ncols
            blocks = []
            cur = []
            for sg in segs:
                if cur and (sg[0] + sg[1] - cur[0][0] > 512):
                    blocks.append(cur)
                    cur = []
                cur.append(sg)
            if cur:
                blocks.append(cur)

            with ExitStack() as ps_:
                sb = lambda n, s, d: ps_.enter_context(nc.sbuf_tensor(uname(n), list(s), d))
                pp = lambda n, s, d: ps_.enter_context(nc.psum_tensor(uname(n), list(s), d))
                wr = Ring([sb("a_w%d" % i, [128, 16, 512], BF16) for i in range(2)])
                stT = sb("a_stT", [128, 4, S_LEN], BF16)
                stK = sb("a_stK", [128, NT, 512], BF16)
                stG = sb("a_stG", [128, NT, 32], F32)
                stT_b = [[Buf() for _ in range(4)] for _ in range(4)]
                stK_b = [[Buf() for _ in range(4)] for _ in range(4)]
                stG_b = [Buf() for _ in range(4)]
                gq = sb("a_gq", [128, 128], F32)
                gk = sb("a_gk", [128, 128], F32)
                b_gain = Buf()
                S.dma("sp", A("dma_start", out=gq[:], in_=gq_dram[:, :]), writes=[b_gain])
                S.dma("sp", A("dma_start", out=gk[:], in_=gk_dram[:, :]), writes=[b_gain])
                kdec = None
                if kdec_dram is not None:
                    kdec = sb("a_kdec", [128, RH], F32)
                    S.dma("sp", A("dma_start", out=kdec[:], in_=kdec_dram[:, :]), writes=[b_gain])
                psr = Ring([pp("a_ps%d" % i, [128, 512], F32) for i in range(2)])
                ptr = Ring([pp("a_pt%d" % i, [128, 4, 128], BF16) for i in range(2)])
                xsr = Ring([sb("a_xs%d" % i, [128, 512], F32) for i in range(2)])
                sqr = Ring([sb("a_sq%d" % i, [128, 512], F32) for i in range(2)])
                msr = Ring([sb("a_ms%d" % i, [128, 4], F32) for i in range(2)])
                rsr = Ring([sb("a_rs%d" % i, [128, 4], F32) for i in range(2)])
                tmr = [Ring([sb("a_t%d_%d" % (k, i), [128, 256], F32) for i in range(2)]) for k in range(4)]
                rbr = Ring([sb("a_rb%d" % i, [128, 512], BF16) for i in range(2)])

                def load_w(bi):
                    blk = blocks[bi]
                    c0 = blk[0][0]
                    wd = blk[-1][0] + blk[-1][1] - c0
                    wt, b_w = wr.next()
                    for hf in range(2):
                        S.dma("pool", A("dma_start", out=wt[:, hf * 8:(hf + 1) * 8, 0:wd],
                                        in_=w_dram[hf * 1024:(hf + 1) * 1024, c0:c0 + wd].rearrange("(k p) n -> p k n", p=128)),
                              writes=[b_w])
                    return wt, b_w, c0, wd

                def do_mm(t, wt, b_w, wd):
                    ps, b_ps = psr.next()
                    S.op("pe", MM([dict(out=ps[:, 0:wd], lhsT=hT[:, k, t * 128:(t + 1) * 128], rhs=wt[:, k, 0:wd])
                                   for k in range(16)]), reads=[hT_b[t], b_w], writes=[b_ps])
                    return ps, b_ps

                def to_T(rb, b_rb, n, t, slot0):
                    pt, b_pt = ptr.next()
                    S.op("pe", TR([dict(out=pt[:, jj, :], in_=rb[:, jj * 128:(jj + 1) * 128], identity=ident[:])
                                   for jj in range(n)]), reads=[b_rb, b_const], writes=[b_pt])
                    S.op("act", A("copy", out=stT[:, slot0:slot0 + n, t * 128:(t + 1) * 128], in_=pt[:, 0:n, :]),
                         reads=[b_pt], writes=[stT_b[t // 4][s_] for s_ in range(slot0, slot0 + n)])

                def post(bi, t, c0, ps, b_ps):
                    blk = blocks[bi]
                    runs = []
                    for sg in blk:
                        if runs and runs[-1][0] == sg[2]:
                            runs[-1][1].append(sg)
                        else:
                            runs.append((sg[2], [sg]))
                    tg = t // 4
                    for si, sgs in runs:
                        sec = sections[si]
                        kind = sec["kind"]
                        lc0 = sgs[0][0] - c0
                        n = len(sgs)
                        w_ = sum(s_[1] for s_ in sgs)
                        slot0 = lc0 // 128
                        h0 = sgs[0][3]
                        kb = [stK_b[tg][s_] for s_ in range(slot0, slot0 + n)]
                        tb = [stT_b[tg][s_] for s_ in range(slot0, slot0 + n)]
                        if kind in ("plain", "silu"):
                            fn = AF.Copy if kind == "plain" else AF.Silu
                            S.op("act", A("activation", out=stK[:, t, lc0:lc0 + w_], in_=ps[:, lc0:lc0 + w_], func=fn),
                                 reads=[b_ps], writes=kb)
                        elif kind == "sigmoid":
                            S.op("act", A("activation", out=stG[:, t, 0:w_], in_=ps[:, lc0:lc0 + w_], func=AF.Sigmoid),
                                 reads=[b_ps], writes=[stG_b[tg]])
                        elif kind == "plainT":
                            rb, b_rb = rbr.next()
                            S.op("act", A("copy", out=rb[:, 0:w_], in_=ps[:, lc0:lc0 + w_]), reads=[b_ps], writes=[b_rb])
                            to_T(rb, b_rb, n, t, slot0)
                        elif kind in ("rope", "rope_k", "normrope_q", "normrope_k"):
                            xs, b_xs = xsr.next()
                            S.op("act", A("copy", out=xs[:, 0:w_], in_=ps[:, lc0:lc0 + w_]), reads=[b_ps], writes=[b_xs])
                            if kind.startswith("normrope"):
                                g_t = gq if kind == "normrope_q" else gk
                                sq, b_sq = sqr.next()
                                ms, b_ms = msr.next()
                                rs, b_rs = rsr.next()
                                S.op("act", A("activation", out=sq[:, 0:w_], in_=ps[:, lc0:lc0 + w_], func=AF.Square),
                                     reads=[b_ps], writes=[b_sq])
                                S.op("dve", A("tensor_reduce", out=ms[:, 0:n],
                                              in_=sq[:, 0:n * 128].rearrange("p (h d) -> p h d", d=128),
                                              axis=AX.X, op=ALU.add), reads=[b_sq], writes=[b_ms])
                                rstd_ops(ms[:, 0:n], rs[:, 0:n], b_ms, b_rs, 1.0 / 128)
                                for jj in range(n):
                                    S.op("dve", A("scalar_tensor_tensor", out=xs[:, jj * 128:(jj + 1) * 128],
                                                  in0=xs[:, jj * 128:(jj + 1) * 128], scalar=rs[:, jj:jj + 1], in1=g_t[:],
                                                  op0=ALU.mult, op1=ALU.mult),
                                         reads=[b_xs, b_rs, b_gain], writes=[b_xs])
                            rb, b_rb = rbr.next()
                            xv = xs[:, 0:n * 128].rearrange("p (h d) -> p h d", d=128)
                            rv = rb[:, 0:n * 128].rearrange("p (h d) -> p h d", d=128)
                            cb = cosT[:, t, :].unsqueeze(1).to_broadcast([128, n, 64])
                            sbb = sinT[:, t, :].unsqueeze(1).to_broadcast([128, n, 64])
                            rope_ops(xv, rv, b_xs, b_rb, cb, sbb, b_const, [r_.next() for r_ in tmr], 128, n)
                            to_T(rb, b_rb, n, t, slot0)
                            if kind == "rope_k":
                                S.op("pool", A("tensor_tensor",
                                               out=stK[:, t, lc0:lc0 + n * 128].rearrange("p (h d) -> p h d", d=128),
                                               in0=rv, in1=kdec[:, h0:h0 + n].unsqueeze(2).to_broadcast([128, n, 128]),
                                               op=ALU.mult), reads=[b_rb, b_gain], writes=kb)
                        else:
                            raise ValueError(kind)
                        if t % 4 == 3:
                            tok0 = (t - 3) * 128
                            if kind in ("rope", "rope_k", "normrope_q", "normrope_k", "plainT"):
                                dst = FT[sec["ft"]]
                                S.dma("sp", A("dma_start",
                                              out=dst[h0:h0 + n, :, tok0:tok0 + 512].rearrange("h d s -> d h s"),
                                              in_=stT[:, slot0:slot0 + n, tok0:tok0 + 512]),
                                      reads=tb, writes=[DB[sec["ft"]]])
                            if kind in ("plain", "silu", "rope_k"):
                                dst = TK[sec["tk"]]
                                cc0 = h0 * 128
                                S.dma("sp", A("dma_start",
                                              out=dst[t - 3:t + 1, :, cc0:cc0 + w_].rearrange("t p c -> p t c"),
                                              in_=stK[:, t - 3:t + 1, lc0:lc0 + w_]),
                                      reads=kb, writes=[DB[sec["tk"]]])
                            if kind == "sigmoid":
                                dst = TK[sec["tk"]]
                                S.dma("sp", A("dma_start",
                                              out=dst[t - 3:t + 1, :, 0:w_].rearrange("t p c -> p t c"),
                                              in_=stG[:, t - 3:t + 1, 0:w_]),
                                      reads=[stG_b[tg]], writes=[DB[sec["tk"]]])

                nb = len(blocks)
                wts = {0: load_w(0)}
                prev = None
                for bi in range(nb):
                    wt, b_w, c0, wd = wts[bi]
                    for t in range(NT):
                        if t == 0 and bi + 1 < nb:
                            wts[bi + 1] = load_w(bi + 1)
                        ps, b_ps = do_mm(t, wt, b_w, wd)
                        if prev is not None:
                            post(*prev)
                        prev = (bi, t, c0, ps, b_ps)
                post(*prev)
            S.barrier()

        def phase_ret():
            GH = min(4, RH)
            with ExitStack() as ps_:
                sb = lambda n, s, d: ps_.enter_context(nc.sbuf_tensor(uname(n), list(s), d))
                pp = lambda n, s, d: ps_.enter_context(nc.psum_tensor(uname(n), list(s), d))
                DT = sb("r_DT", [128, RH, 128], F32)
                qdec = sb("r_qdec", [128, RH, 128], F32)
                b_rc = Buf()
                S.dma("sp", A("dma_start", out=DT[:], in_=c_DT[:, :, :]), writes=[b_rc])
                S.dma("sp", A("dma_start", out=qdec[:], in_=c_qdec[:, :, :]), writes=[b_rc])
                cd = sb("r_cd", [128, RH], F32)
                S.dma("sp", A("dma_start", out=cd[:], in_=c_cdec[:, :]), writes=[b_rc])
                mk = lambda nm, shp, dt: [(sb("r_%s%d" % (nm, i), shp, dt), Buf()) for i in range(GH)]
                QT = mk("QT", [128, S_LEN], BF16)
                KT = mk("KT", [128, S_LEN], BF16)
                Qd = mk("Qd", [128, S_LEN], BF16)
                Kd = mk("Kd", [128, NT, 128], BF16)
                V = mk("V", [128, NT, 128], BF16)
                Rg = mk("Rg", [128, NT, 128], BF16)
                st = mk("st", [128, 128], F32)
                stb = mk("stb", [128, 128], BF16)
                ybuf = sb("r_y", [128, NT, GH * 128], BF16)
                b_y = Buf()
                junk = sb("r_junk", [128, 128], BF16)
                b_junk = Buf()
                sbank = Ring([pp("r_ps%d" % i, [128, 4, 128], F32) for i in range(2)])
                obank = Ring([pp("r_po%d" % i, [128, 4, 128], F32) for i in range(2)])
                kbank = Ring([pp("r_pk%d" % i, [128, 4, 128], F32) for i in range(2)])
                wring = Ring([sb("r_w%d" % i, [128, 4, 128], BF16) for i in range(2)])
                ssr = Ring([sb("r_ss%d" % i, [128, 4], F32) for i in range(2)])
                rsr = Ring([sb("r_rs%d" % i, [128, 4], F32) for i in range(2)])
                for g0 in range(0, RH, GH):
                    heads = list(range(g0, min(RH, g0 + GH)))
                    for i, h in enumerate(heads):
                        S.dma("sp", A("dma_start", out=QT[i][0][:], in_=FT["QrT"][h]), reads=[DB["QrT"]], writes=[QT[i][1]])
                        S.dma("sp", A("dma_start", out=KT[i][0][:], in_=FT["KrT"][h]), reads=[DB["KrT"]], writes=[KT[i][1]])
                        for (dstl, nm) in ((Kd, "Kd"), (V, "Vr"), (Rg, "Rg")):
                            S.dma("sp", A("dma_start", out=dstl[i][0][:],
                                          in_=TK[nm][:, :, h * 128:(h + 1) * 128].rearrange("t p c -> p t c")),
                                  reads=[DB[nm]], writes=[dstl[i][1]])
                        S.op("dve", A("tensor_tensor", out=Qd[i][0][:].rearrange("p (n c) -> p n c", c=128),
                                      in0=QT[i][0][:].rearrange("p (n c) -> p n c", c=128),
                                      in1=qdec[:, h, :].unsqueeze(1).to_broadcast([128, NT, 128]), op=ALU.mult),
                             reads=[QT[i][1], b_rc], writes=[Qd[i][1]])
                    nh_ = len(heads)
                    for n in range(NT):
                        cs = slice(n * 128, (n + 1) * 128)
                        ps_s, b_s = sbank.next()
                        S.op("pe", MMRAW([dict(out=ps_s[:, i, :], lhsT=KT[i][0][:, cs], rhs=QT[i][0][:, cs], start=True, stop=True)
                                          for i in range(nh_)]),
                             reads=[KT[i][1] for i in range(nh_)] + [QT[i][1] for i in range(nh_)], writes=[b_s])
                        wT, b_wT = wring.next()
                        for i, h in enumerate(heads):
                            S.op("dve", A("tensor_tensor", out=wT[:, i, :], in0=ps_s[:, i, :], in1=DT[:, h, :], op=ALU.mult),
                                 reads=[b_s, b_rc], writes=[b_wT])
                        ps_o, b_o = obank.next()
                        lst = []
                        rd = [b_wT]
                        for i in range(nh_):
                            lst.append(dict(out=ps_o[:, i, :], lhsT=wT[:, i, :], rhs=V[i][0][:, n, :], start=True, stop=(n == 0)))
                            rd.append(V[i][1])
                            if n > 0:
                                lst.append(dict(out=ps_o[:, i, :], lhsT=Qd[i][0][:, cs], rhs=stb[i][0][:], start=False, stop=True))
                                rd += [Qd[i][1], stb[i][1]]
                        S.op("pe", MMRAW(lst), reads=rd, writes=[b_o])
                        if n < NT - 1:
                            ps_k, b_k = kbank.next()
                            S.op("pe", MMRAW([dict(out=ps_k[:, i, :], lhsT=Kd[i][0][:, n, :], rhs=V[i][0][:, n, :], start=True, stop=True)
                                              for i in range(nh_)]),
                                 reads=[Kd[i][1] for i in range(nh_)] + [V[i][1] for i in range(nh_)], writes=[b_k])
                            for i, h in enumerate(heads):
                                if n == 0:
                                    S.op("dve", A("tensor_copy", out=st[i][0][:], in_=ps_k[:, i, :]), reads=[b_k], writes=[st[i][1]])
                                else:
                                    S.op("dve", A("scalar_tensor_tensor", out=st[i][0][:], in0=st[i][0][:], scalar=cd[:, h:h + 1],
                                                  in1=ps_k[:, i, :], op0=ALU.mult, op1=ALU.add),
                                         reads=[b_k, st[i][1], b_rc], writes=[st[i][1]])
                                S.op("act", A("copy", out=stb[i][0][:], in_=st[i][0][:]), reads=[st[i][1]], writes=[stb[i][1]])
                        ss, b_ss = ssr.next()
                        rs, b_rs = rsr.next()
                        for i in range(nh_):
                            S.op("act", A("activation", out=junk[:], in_=ps_o[:, i, :], func=AF.Square, accum_out=ss[:, i:i + 1]),
                                 reads=[b_o], writes=[b_junk, b_ss])
                        rstd_ops(ss[:, 0:nh_], rs[:, 0:nh_], b_ss, b_rs, 1.0 / 128)
                        for i in range(nh_):
                            S.op("dve", A("scalar_tensor_tensor", out=ybuf[:, n, i * 128:(i + 1) * 128], in0=ps_o[:, i, :],
                                          scalar=rs[:, i:i + 1], in1=Rg[i][0][:, n, :], op0=ALU.mult, op1=ALU.mult),
                                 reads=[b_o, b_rs, Rg[i][1]], writes=[b_y])
                    S.dma("sp", A("dma_start",
                                  out=TK["Y0"][:, :, g0 * 128:(g0 + nh_) * 128].rearrange("t p c -> p t c"),
                                  in_=ybuf[:, :, 0:nh_ * 128]), reads=[b_y], writes=[DB["Y0"]])
            S.barrier()

        def attention(mode, QT, b_QT, KT, b_KT, V_of, b_V, dv1, rings, finalize, extra=None):
            sring, pring, accs, E, selT, b_sel, tri, anti, b_msk = rings
            for c in range(4):
                kts = range(max(0, 4 * c - 4), 4 * c + 4) if mode == "win" else range(0, 4 * c + 4)
                for kt in kts:
                    lo = max(4 * c, kt)
                    hi = min(4 * c + 3, kt + 4) if mode == "win" else 4 * c + 3
                    cl, ch = (lo - 4 * c) * 128, (hi - 4 * c + 1) * 128
                    q0 = 4 * c * 128
                    ps_s, b_s = sring.next()
                    lst = [dict(out=ps_s[:, cl:ch], lhsT=KT[:, kt * 128:(kt + 1) * 128], rhs=QT[:, q0 + cl:q0 + ch])]
                    rd = [b_KT, b_QT, b_msk, b_const]
                    if mode == "slc":
                        lst.append(dict(out=ps_s[:, cl:ch], lhsT=E[:, kt, :], rhs=selT[:, q0 + cl:q0 + ch]))
                        rd.append(b_sel)
                    if lo == kt:
                        d0 = (kt - 4 * c) * 128
                        lst.append(dict(out=ps_s[:, d0:d0 + 128], lhsT=ident[:], rhs=tri[:]))
                    if mode == "win" and lo <= kt + 4 <= hi:
                        d0 = (kt + 4 - 4 * c) * 128
                        lst.append(dict(out=ps_s[:, d0:d0 + 128], lhsT=ident[:], rhs=anti[:]))
                    S.op("pe", MM(lst), reads=rd, writes=[b_s])
                    pT, b_pT = pring.next()
                    S.op("act", A("activation", out=pT[:, cl:ch], in_=ps_s[:, cl:ch], func=AF.Exp, scale=SCALE),
                         reads=[b_s], writes=[b_pT])
                    for qt in range(lo, hi + 1):
                        acc, b_acc = accs[qt % 4]
                        first = (kt == (max(0, qt - 4) if mode == "win" else 0))
                        last = (kt == qt)
                        d0 = (qt - 4 * c) * 128
                        S.op("pe", MMRAW([dict(out=acc[:, 0:dv1], lhsT=pT[:, d0:d0 + 128], rhs=V_of(kt), start=first, stop=last)]),
                             reads=[b_pT, b_V], writes=[b_acc])
                        if last:
                            finalize(qt, acc, b_acc)

        def phase_nsa():
            with ExitStack() as ps_:
                sb = lambda n, s, d: ps_.enter_context(nc.sbuf_tensor(uname(n), list(s), d))
                pp = lambda n, s, d: ps_.enter_context(nc.psum_tensor(uname(n), list(s), d))
                b_k = Buf()
                cmaskT = sb("s_cmask", [127, S_LEN], BF16)
                E = sb("s_E", [32, NT, 128], BF16)
                tri = sb("s_tri", [128, 128], BF16)
                anti = sb("s_anti", [128, 128], BF16)
                M1 = sb("s_M1", [128, NT, 32], F32)
                M2 = sb("s_M2", [128, NT, 32], F32)
                cosc = sb("s_cosc", [127, 64], F32)
                sinc = sb("s_sinc", [127, 64], F32)
                gk = sb("s_gk", [128, 128], F32)
                Gl = sb("s_Gl", [128, NT, 3 * NH], F32)
                for (dst, src) in ((cmaskT, c_cmaskT[:, :]), (E, c_E[:, :, :]), (tri, c_tri[:, :]), (anti, c_anti[:, :]),
                                   (M1, c_M1[:, :, :]), (M2, c_M2[:, :, :]), (cosc, c_cosc[:, :]), (sinc, c_sinc[:, :]),
                                   (gk, g_k0[:, :])):
                    S.dma("sp", A("dma_start", out=dst[:], in_=src), writes=[b_k])
                S.dma("sp", A("dma_start", out=Gl[:], in_=TK["Gl"].rearrange("t p c -> p t c")), reads=[DB["Gl"]], writes=[b_k])
                W1 = {}
                W2 = {}
                peT = {}
                for kv in ("k", "v"):
                    W1[kv] = sb("s_W1" + kv, [128, 32, 128], BF16)
                    W2[kv] = sb("s_W2" + kv, [128, 128], BF16)
                    peT[kv] = sb("s_pe" + kv, [128, 32], BF16)
                    S.dma("pool", A("dma_start", out=W1[kv][:], in_=w1_in[kv].rearrange("(j d) f -> d j f", d=128)), writes=[b_k])
                    S.dma("pool", A("dma_start", out=W2[kv][:], in_=w2_in[kv][:, :]), writes=[b_k])
                    S.dma("pool", A("dma_start", out=peT[kv][:], in_=pe_in[kv][:, :]), writes=[b_k])
                XcT = {"k": (sb("s_KcT", [128, S_LEN], BF16), Buf()), "v": (sb("s_VcT", [128, S_LEN], BF16), Buf())}
                QTs = [(sb("s_QT%d" % i, [128, S_LEN], BF16), Buf()) for i in range(GQ)]
                KsT = (sb("s_KsT", [128, S_LEN], BF16), Buf())
                KwT = (sb("s_KwT", [128, S_LEN], BF16), Buf())
                Vs = (sb("s_Vs", [128, NT, 129], BF16), Buf())
                Vw = (sb("s_Vw", [128, NT, 129], BF16), Buf())
                S.op("dve", A("memset", ap=Vs[0][:, :, 128:129], constant=1.0), writes=[Vs[1]])
                S.op("dve", A("memset", ap=Vw[0][:, :, 128:129], constant=1.0), writes=[Vw[1]])
                Ng = (sb("s_Ng", [128, NT, GQ * 128], BF16), Buf())
                nacc = sb("s_nacc", [128, NT, GQ, 128], F32)
                nacc_b = [[Buf() for _ in range(GQ)] for _ in range(NT)]
                imp = sb("s_imp", [128, NT, 32], F32)
                imp_b = [Buf() for _ in range(NT)]
                selT = sb("s_selT", [32, S_LEN], BF16)
                b_sel = Buf()
                ybuf = sb("s_y", [128, NT, GQ * 128], BF16)
                b_y = Buf()
                kcmpT = (sb("s_kcmpT", [128, 128], BF16), Buf())
                cmpV = (sb("s_cmpV", [127, 161], BF16), Buf())
                cb = (sb("s_cb", [128, 1], F32), Buf())
                hT_ = (sb("s_hT", [128, 128], BF16), Buf())
                xs = (sb("s_xs", [127, 128], F32), Buf())
                kc = (sb("s_kc", [127, 128], BF16), Buf())
                junk = (sb("s_junk", [127, 128], BF16), Buf())
                ms = (sb("s_ms", [127, 1], F32), Buf())
                rs1 = (sb("s_rs1", [127, 1], F32), Buf())
                tmps = [(sb("s_t%d" % i, [127, 64], F32), Buf()) for i in range(4)]
                sring = Ring([pp("s_ps%d" % i, [128, 512], F32) for i in range(2)])
                accs = [(pp("s_acc%d" % i, [128, 512], F32), Buf()) for i in range(4)]
                mring = Ring([pp("s_pm%d" % i, [128, 512], F32) for i in range(2)])
                pring = Ring([sb("s_pT%d" % i, [128, 512], BF16) for i in range(3)])
                rsr = Ring([sb("s_rs%d" % i, [128, 1], F32) for i in range(4)])
                cfr = Ring([sb("s_cf%d" % i, [128, 1], F32) for i in range(4)])
                scr = Ring([sb("s_sc%d" % i, [128, 32], F32) for i in range(2)])
                sc2r = Ring([sb("s_sc2%d" % i, [128, 32], F32) for i in range(2)])
                m8r = Ring([sb("s_m8%d" % i, [128, 16], F32) for i in range(2)])
                sbr = Ring([sb("s_sb%d" % i, [128, 32], BF16) for i in range(2)])
                rings = (sring, pring, accs, E, selT, b_sel, tri, anti, b_k)

                for g in range(NG):
                    S.dma("sp", A("dma_start", out=XcT["k"][0][:], in_=FT["KcT"][g]), reads=[DB["KcT"]], writes=[XcT["k"][1]])
                    S.dma("sp", A("dma_start", out=XcT["v"][0][:], in_=FT["VcT"][g]), reads=[DB["VcT"]], writes=[XcT["v"][1]])
                    for r in range(GQ):
                        S.dma("sp", A("dma_start", out=QTs[r][0][:], in_=FT["QnT"][g * GQ + r]), reads=[DB["QnT"]], writes=[QTs[r][1]])
                    S.dma("sp", A("dma_start", out=KsT[0][:], in_=FT["KsT"][g]), reads=[DB["KsT"]], writes=[KsT[1]])
                    S.dma("sp", A("dma_start", out=KwT[0][:], in_=FT["KwT"][g]), reads=[DB["KwT"]], writes=[KwT[1]])
                    S.dma("sp", A("dma_start", out=Vs[0][:, :, 0:128],
                                  in_=TK["Vs"][:, :, g * 128:(g + 1) * 128].rearrange("t p c -> p t c")),
                          reads=[DB["Vs"]], writes=[Vs[1]])
                    S.dma("sp", A("dma_start", out=Vw[0][:, :, 0:128],
                                  in_=TK["Vw"][:, :, g * 128:(g + 1) * 128].rearrange("t p c -> p t c")),
                          reads=[DB["Vw"]], writes=[Vw[1]])
                    S.dma("sp", A("dma_start", out=Ng[0][:],
                                  in_=TK["Ng"][:, :, g * GQ * 128:(g + 1) * GQ * 128].rearrange("t p c -> p t c")),
                          reads=[DB["Ng"]], writes=[Ng[1]])
                    S.dma("sp", A("dma_start", out=cmpV[0][:], in_=c_cmpv[:, :]), writes=[cmpV[1]])
                    for kv in ("k", "v"):
                        pm, b_pm = mring.next()
                        S.op("pe", MM([dict(out=pm[:, 0:1], lhsT=W1[kv][:, j, :], rhs=peT[kv][:, j:j + 1]) for j in range(32)]),
                             reads=[b_k], writes=[b_pm])
                        S.op("act", A("copy", out=cb[0][:], in_=pm[:, 0:1]), reads=[b_pm], writes=[cb[1]])
                        pm, b_pm = mring.next()
                        S.op("pe", MM([dict(out=pm[:, 0:127], lhsT=W1[kv][:, j, :], rhs=XcT[kv][0][:, j:j + 2017:16])
                                       for j in range(32)]), reads=[b_k, XcT[kv][1]], writes=[b_pm])
                        S.op("act", A("activation", out=hT_[0][:, 0:127], in_=pm[:, 0:127], func=AF.Silu, bias=cb[0][:, 0:1], scale=1.0),
                             reads=[b_pm, cb[1]], writes=[hT_[1]])
                        pm, b_pm = mring.next()
                        S.op("pe", MM([dict(out=pm[0:127, 0:128], lhsT=hT_[0][:, 0:127], rhs=W2[kv][:])]),
                             reads=[hT_[1], b_k], writes=[b_pm])
                        if kv == "v":
                            S.op("act", A("copy", out=cmpV[0][:, 0:128], in_=pm[0:127, 0:128]), reads=[b_pm], writes=[cmpV[1]])
                        else:
                            S.op("act", A("copy", out=xs[0][:], in_=pm[0:127, 0:128]), reads=[b_pm], writes=[xs[1]])
                            S.op("act", A("activation", out=junk[0][:], in_=pm[0:127, 0:128], func=AF.Square, accum_out=ms[0][:, 0:1]),
                                 reads=[b_pm], writes=[junk[1], ms[1]])
                            rstd_ops(ms[0][:], rs1[0][:], ms[1], rs1[1], 1.0 / 128)
                            S.op("dve", A("scalar_tensor_tensor", out=xs[0][:], in0=xs[0][:], scalar=rs1[0][:, 0:1], in1=gk[0:127, :],
                                          op0=ALU.mult, op1=ALU.mult), reads=[xs[1], rs1[1], b_k], writes=[xs[1]])
                            rope_ops(xs[0][:].unsqueeze(1), kc[0][:].unsqueeze(1), xs[1], kc[1],
                                     cosc[:].unsqueeze(1), sinc[:].unsqueeze(1), b_k, tmps, 127, 1)
                            pm2, b_pm2 = mring.next()
                            ptv = pm2[:].bitcast(BF16)
                            S.op("pe", TR([dict(out=ptv[:, 0:127], in_=kc[0][:], identity=ident[0:127, 0:127])]),
                                 reads=[kc[1], b_const], writes=[b_pm2])
                            S.op("act", A("copy", out=kcmpT[0][:, 0:127], in_=ptv[:, 0:127]), reads=[b_pm2], writes=[kcmpT[1]])
                    for r in range(GQ):
                        h = g * GQ + r
                        QT, b_QT = QTs[r]
                        for c in range(4):
                            ps_s, b_s = sring.next()
                            S.op("pe", MM([dict(out=ps_s[0:127, :], lhsT=kcmpT[0][:, 0:127], rhs=QT[:, c * 512:(c + 1) * 512])]),
                                 reads=[kcmpT[1], b_QT], writes=[b_s])
                            pT, b_pT = pring.next()
                            S.op("act", A("activation", out=pT[0:127, :], in_=ps_s[0:127, :], func=AF.Exp, scale=SCALE),
                                 reads=[b_s], writes=[b_pT])
                            S.op("dve", A("tensor_tensor", out=pT[0:127, :], in0=pT[0:127, :], in1=cmaskT[:, c * 512:(c + 1) * 512],
                                          op=ALU.mult), reads=[b_pT, b_k], writes=[b_pT])
                            for q4 in range(4):
                                qt = c * 4 + q4
                                acc, b_acc = accs[qt % 4]
                                S.op("pe", MM([dict(out=acc[:, 0:161], lhsT=pT[0:127, q4 * 128:(q4 + 1) * 128], rhs=cmpV[0][:])]),
                                     reads=[b_pT, cmpV[1]], writes=[b_acc])
                                rs, b_rs = rsr.next()
                                cf, b_cf = cfr.next()
                                S.op("dve", A("tensor_scalar_max", out=rs[:], in0=acc[:, 128:129], scalar1=1e-30), reads=[b_acc], writes=[b_rs])
                                S.op("dve", A("reciprocal", out=rs[:], in_=rs[:]), reads=[b_rs], writes=[b_rs])
                                S.op("dve", A("tensor_tensor", out=cf[:], in0=rs[:], in1=Gl[:, qt, h:h + 1], op=ALU.mult),
                                     reads=[b_rs, b_k], writes=[b_cf])
                                S.op("dve", A("tensor_scalar_mul", out=nacc[:, qt, r, :], in0=acc[:, 0:128], scalar1=cf[:, 0:1]),
                                     reads=[b_acc, b_cf], writes=[nacc_b[qt][r]])
                                if r == 0:
                                    S.op("dve", A("tensor_scalar_mul", out=imp[:, qt, :], in0=acc[:, 129:161], scalar1=rs[:, 0:1]),
                                         reads=[b_acc, b_rs], writes=[imp_b[qt]])
                                else:
                                    S.op("dve", A("scalar_tensor_tensor", out=imp[:, qt, :], in0=acc[:, 129:161], scalar=rs[:, 0:1],
                                                  in1=imp[:, qt, :], op0=ALU.mult, op1=ALU.add),
                                         reads=[b_acc, b_rs, imp_b[qt]], writes=[imp_b[qt]])
                    for qt in range(NT):
                        sc, b_sc = scr.next()
                        sc2, b_sc2 = sc2r.next()
                        m8, b_m8 = m8r.next()
                        selb, b_sb = sbr.next()
                        S.op("dve", A("tensor_tensor", out=sc[:], in0=imp[:, qt, :], in1=M1[:, qt, :], op=ALU.mult),
                             reads=[imp_b[qt], b_k], writes=[b_sc])
                        S.op("dve", A("tensor_tensor", out=sc[:], in0=sc[:], in1=M2[:, qt, :], op=ALU.add),
                             reads=[b_sc, b_k], writes=[b_sc])
                        S.op("dve", A("max", out=m8[:, 0:8], in_=sc[:]), reads=[b_sc], writes=[b_m8])
                        S.op("dve", A("match_replace", out=sc2[:], in_to_replace=m8[:, 0:8], in_values=sc[:], imm_value=-3e9),
                             reads=[b_sc, b_m8], writes=[b_sc2])
                        S.op("dve", A("max", out=m8[:, 8:16], in_=sc2[:]), reads=[b_sc2, b_m8], writes=[b_m8])
                        S.op("dve", A("tensor_scalar", out=selb[:], in0=sc[:], scalar1=m8[:, 15:16], scalar2=1.0,
                                      op0=ALU.is_ge, op1=ALU.subtract), reads=[b_sc, b_m8], writes=[b_sb])
                        pm, b_pm = mring.next()
                        ptv = pm[:].bitcast(BF16)
                        S.op("pe", TR([dict(out=ptv[0:32, 0:128], in_=selb[:], identity=ident[:])]),
                             reads=[b_sb, b_const], writes=[b_pm])
                        S.op("act", A("copy", out=selT[:, qt * 128:(qt + 1) * 128], in_=ptv[0:32, 0:128]), reads=[b_pm], writes=[b_sel])
                    for r in range(GQ):
                        h = g * GQ + r
                        QT, b_QT = QTs[r]
                        for (mode, br, KT_, V_) in (("slc", 1, KsT, Vs), ("win", 2, KwT, Vw)):
                            def fin(qt, acc, b_acc, br=br, h=h, r=r):
                                rs, b_rs = rsr.next()
                                cf, b_cf = cfr.next()
                                S.op("dve", A("reciprocal", out=rs[:], in_=acc[:, 128:129]), reads=[b_acc], writes=[b_rs])
                                S.op("dve", A("tensor_tensor", out=cf[:], in0=rs[:], in1=Gl[:, qt, br * NH + h:br * NH + h + 1], op=ALU.mult),
                                     reads=[b_rs, b_k], writes=[b_cf])
                                S.op("dve", A("scalar_tensor_tensor", out=nacc[:, qt, r, :], in0=acc[:, 0:128], scalar=cf[:, 0:1],
                                              in1=nacc[:, qt, r, :], op0=ALU.mult, op1=ALU.add),
                                     reads=[b_acc, b_cf, nacc_b[qt][r]], writes=[nacc_b[qt][r]])
                            attention(mode, QT, b_QT, KT_[0], KT_[1], (lambda kt, V_=V_: V_[0][:, kt, :]), V_[1], 129, rings, fin)
                    for qt in range(NT):
                        S.op("pool", A("tensor_tensor", out=ybuf[:, qt, :], in0=nacc[:, qt, :, :].rearrange("p r d -> p (r d)"),
                                       in1=Ng[0][:, qt, :], op=ALU.mult),
                             reads=[nacc_b[qt][r] for r in range(GQ)] + [Ng[1]], writes=[b_y])
                    c0 = RH * 128 + g * GQ * 128
                    S.dma("sp", A("dma_start", out=TK["Y0"][:, :, c0:c0 + GQ * 128].rearrange("t p c -> p t c"), in_=ybuf[:]),
                          reads=[b_y], writes=[DB["Y0"]])
            S.barrier()

        def phase_diff():
            with ExitStack() as ps_:
                sb = lambda n, s, d: ps_.enter_context(nc.sbuf_tensor(uname(n), list(s), d))
                pp = lambda n, s, d: ps_.enter_context(nc.psum_tensor(uname(n), list(s), d))
                b_k = Buf()
                tri = sb("d_tri", [128, 128], BF16)
                S.dma("sp", A("dma_start", out=tri[:], in_=c_tri[:, :]), writes=[b_k])
                lam_t = sb("d_lam", [128, 4, 128], F32)
                S.dma("sp", A("dma_start", out=lam_t[:], in_=lam_in[:, :, :]), writes=[b_k])
                prod = sb("d_prod", [128, 2, 128], F32)
                sums = sb("d_sums", [128, 2], F32)
                nlam = sb("d_nlam", [128, 1], F32)
                b_l = Buf()
                S.op("dve", A("tensor_tensor", out=prod[:, 0, :], in0=lam_t[:, 0, :], in1=lam_t[:, 1, :], op=ALU.mult), reads=[b_k], writes=[b_l])
                S.op("dve", A("tensor_tensor", out=prod[:, 1, :], in0=lam_t[:, 2, :], in1=lam_t[:, 3, :], op=ALU.mult), reads=[b_k, b_l], writes=[b_l])
                S.op("dve", A("tensor_reduce", out=sums[:], in_=prod[:], axis=AX.X, op=ALU.add), reads=[b_l], writes=[b_l])
                S.op("act", A("activation", out=sums[:], in_=sums[:], func=AF.Exp), reads=[b_l], writes=[b_l])
                S.op("dve", A("tensor_tensor", out=nlam[:], in0=sums[:, 1:2], in1=sums[:, 0:1], op=ALU.subtract), reads=[b_l], writes=[b_l])
                S.op("dve", A("tensor_scalar_add", out=nlam[:], in0=nlam[:], scalar1=-LAMBDA_INIT), reads=[b_l], writes=[b_l])
                NS = 2
                slots = []
                for i in range(NS):
                    sl = dict(
                        Q=[(sb("d_Q%d_%d" % (i, c), [128, S_LEN], BF16), Buf()) for c in range(2)],
                        K=[(sb("d_K%d_%d" % (i, c), [128, S_LEN], BF16), Buf()) for c in range(2)],
                        V=(sb("d_V%d" % i, [128, NT, 257], BF16), Buf()),
                        G=(sb("d_G%d" % i, [128, NT, 256], BF16), Buf()),
                        Y=(sb("d_Y%d" % i, [128, NT, 256], BF16), Buf()),
                    )
                    S.op("dve", A("memset", ap=sl["V"][0][:, :, 256:257], constant=1.0), writes=[sl["V"][1]])
                    slots.append(sl)
                o1 = sb("d_o1", [128, NT, 256], F32)
                o1_b = [Buf() for _ in range(NT)]
                otr = Ring([sb("d_ot%d" % i, [128, 256], F32) for i in range(2)])
                junk = (sb("d_junk", [128, 256], BF16), Buf())
                sring = Ring([pp("d_ps%d" % i, [128, 512], F32) for i in range(2)])
                accs = [(pp("d_acc%d" % i, [128, 512], F32), Buf()) for i in range(4)]
                pring = Ring([sb("d_pT%d" % i, [128, 512], BF16) for i in range(3)])
                rsr = Ring([sb("d_rs%d" % i, [128, 1], F32) for i in range(4)])
                cfr = Ring([sb("d_cf%d" % i, [128, 1], F32) for i in range(4)])
                ssr = Ring([sb("d_ss%d" % i, [128, 1], F32) for i in range(4)])
                r2r = Ring([sb("d_r2%d" % i, [128, 1], F32) for i in range(4)])
                rings = (sring, pring, accs, None, None, None, tri, None, b_k)

                def load(h):
                    sl = slots[h % NS]
                    for c in range(2):
                        S.dma("sp", A("dma_start", out=sl["Q"][c][0][:], in_=FT["Q1T"][2 * h + c]), reads=[DB["Q1T"]], writes=[sl["Q"][c][1]])
                        S.dma("sp", A("dma_start", out=sl["K"][c][0][:], in_=FT["K1T"][2 * h + c]), reads=[DB["K1T"]], writes=[sl["K"][c][1]])
                    S.dma("sp", A("dma_start", out=sl["V"][0][:, :, 0:256],
                                  in_=TK["V1"][:, :, h * 256:(h + 1) * 256].rearrange("t p c -> p t c")),
                          reads=[DB["V1"]], writes=[sl["V"][1]])
                    S.dma("sp", A("dma_start", out=sl["G"][0][:],
                                  in_=TK["G1"][:, :, h * 256:(h + 1) * 256].rearrange("t p c -> p t c")),
                          reads=[DB["G1"]], writes=[sl["G"][1]])

                load(0)
                for h in range(DH):
                    if h + 1 < DH:
                        load(h + 1)
                    sl = slots[h % NS]

                    def fin0(qt, acc, b_acc):
                        rs, b_rs = rsr.next()
                        S.op("dve", A("reciprocal", out=rs[:], in_=acc[:, 256:257]), reads=[b_acc], writes=[b_rs])
                        S.op("dve", A("tensor_scalar_mul", out=o1[:, qt, :], in0=acc[:, 0:256], scalar1=rs[:, 0:1]),
                             reads=[b_acc, b_rs], writes=[o1_b[qt]])

                    def fin1(qt, acc, b_acc, sl=sl):
                        rs, b_rs = rsr.next()
                        cf, b_cf = cfr.next()
                        ot, b_ot = otr.next()
                        ss, b_ss = ssr.next()
                        r2, b_r2 = r2r.next()
                        S.op("dve", A("reciprocal", out=rs[:], in_=acc[:, 256:257]), reads=[b_acc], writes=[b_rs])
                        S.op("dve", A("tensor_tensor", out=cf[:], in0=rs[:], in1=nlam[:], op=ALU.mult), reads=[b_rs, b_l], writes=[b_cf])
                        S.op("dve", A("scalar_tensor_tensor", out=ot[:], in0=acc[:, 0:256], scalar=cf[:, 0:1], in1=o1[:, qt, :],
                                      op0=ALU.mult, op1=ALU.add), reads=[b_acc, b_cf, o1_b[qt]], writes=[b_ot])
                        S.op("act", A("activation", out=junk[0][:], in_=ot[:], func=AF.Square, accum_out=ss[:, 0:1]),
                             reads=[b_ot], writes=[junk[1], b_ss])
                        rstd_ops(ss[:], r2[:], b_ss, b_r2, 1.0 / 256)
                        S.op("dve", A("tensor_scalar_mul", out=r2[:], in0=r2[:], scalar1=float(1.0 - LAMBDA_INIT)), reads=[b_r2], writes=[b_r2])
                        S.op("dve", A("scalar_tensor_tensor", out=sl["Y"][0][:, qt, :], in0=ot[:], scalar=r2[:, 0:1], in1=sl["G"][0][:, qt, :],
                                      op0=ALU.mult, op1=ALU.mult), reads=[b_ot, b_r2, sl["G"][1]], writes=[sl["Y"][1]])

                    for c, fin in ((0, fin0), (1, fin1)):
                        attention("causal", sl["Q"][c][0], sl["Q"][c][1], sl["K"][c][0], sl["K"][c][1],
                                  (lambda kt, sl=sl: sl["V"][0][:, kt, :]), sl["V"][1], 257, rings, fin)
                    S.dma("sp", A("dma_start", out=TK["Y1"][:, :, h * 256:(h + 1) * 256].rearrange("t p c -> p t c"), in_=sl["Y"][0][:]),
                          reads=[sl["Y"][1]], writes=[DB["Y1"]])
            S.barrier()

        def gather(yname):
            if not split:
                return
            yl, yg = YG[yname]
            S.collective(A("collective_compute", kind="AllGather", op=ALU.bypass,
                           replica_groups=[list(range(ncores))], ins=[yl.opt()], outs=[yg.opt()]),
                         cc_inc, reads=[DB[yname]], writes=[DB[yname + "g"]])

        def phase_outproj(yname, w_dram, res_tiles, res_buf, dst_tiles, dst_buf, ncol):
            ncb = ncol // 512
            with ExitStack() as ps_:
                sb = lambda n, s, d: ps_.enter_context(nc.sbuf_tensor(uname(n), list(s), d))
                pp = lambda n, s, d: ps_.enter_context(nc.psum_tensor(uname(n), list(s), d))
                w = sb("c_w", [128, 16, ncol], BF16)
                w_b = [Buf() for _ in range(ncb)]
                for cbk in range(ncb):
                    for hf in range(2):
                        S.dma("pool", A("dma_start", out=w[:, hf * 8:(hf + 1) * 8, cbk * 512:(cbk + 1) * 512],
                                        in_=w_dram[hf * 1024:(hf + 1) * 1024, cbk * 512:(cbk + 1) * 512].rearrange("(k p) n -> p k n", p=128)),
                              writes=[w_b[cbk]])
                yr = Ring([sb("c_y%d" % i, [128, D], BF16) for i in range(2)])
                if split:
                    yidx = sb("c_yidx", [128, 2 * NT], mybir.dt.int32)
                    b_yidx = Buf()
                    S.dma("sp", A("dma_start", out=yidx[:], in_=yidx_in[:, :]), writes=[b_yidx])
                yTr = Ring([sb("c_yT%d" % i, [128, 16, 128], BF16) for i in range(2)])
                xrr = Ring([sb("c_xr%d" % i, [128, ncol], F32) for i in range(2)])
                xor_ = Ring([sb("c_xo%d" % i, [128, ncol], F32) for i in range(2)])
                ptr = Ring([pp("c_pt%d" % i, [128, 8, 128], BF16) for i in range(2)])
                psr = Ring([pp("c_ps%d" % i, [128, 512], F32) for i in range(3)])

                def stage1(t):
                    yt, b_yt = yr.next()
                    if split:
                        yg = YG[yname][1]
                        for rk in range(2):
                            S.dma("pool", A("indirect_dma_start", out=yt[:, rk * YW:(rk + 1) * YW], out_offset=None, in_=yg[:, :],
                                            in_offset=bass.IndirectOffsetOnAxis(ap=yidx[:, 2 * t + rk:2 * t + rk + 1], axis=0)),
                                  reads=[DB[yname + "g"], b_yidx], writes=[b_yt])
                    else:
                        S.dma("sp", A("dma_start", out=yt[:], in_=TK[yname][t]), reads=[DB[yname]], writes=[b_yt])
                    xr, b_xr = xrr.next()
                    S.dma("sp", A("dma_start", out=xr[:], in_=res_tiles[t][:, 0:ncol]),
                          reads=[res_buf] if res_buf is not None else [], writes=[b_xr])
                    yT, b_yT = yTr.next()
                    for half in range(2):
                        pt, b_pt = ptr.next()
                        S.op("pe", TR([dict(out=pt[:, jj, :], in_=yt[:, (half * 8 + jj) * 128:(half * 8 + jj + 1) * 128], identity=ident[:])
                                       for jj in range(8)]), reads=[b_yt, b_const], writes=[b_pt])
                        if half == 0:
                            S.op("act", A("copy", out=yT[:, 0:8, :], in_=pt[:]), reads=[b_pt], writes=[b_yT])
                        else:
                            S.op("dve", A("tensor_copy", out=yT[:, 8:16, :], in_=pt[:]), reads=[b_pt], writes=[b_yT])
                    return yT, b_yT, xr, b_xr

                def stage2(t, yT, b_yT, xr, b_xr):
                    xo, b_xo = xor_.next()
                    for cbk in range(ncb):
                        ps, b_ps = psr.next()
                        S.op("pe", MM([dict(out=ps[:], lhsT=yT[:, k, :], rhs=w[:, k, cbk * 512:(cbk + 1) * 512]) for k in range(16)]),
                             reads=[b_yT, w_b[cbk]], writes=[b_ps])
                        S.op("dve", A("tensor_tensor", out=xo[:, cbk * 512:(cbk + 1) * 512], in0=ps[:], in1=xr[:, cbk * 512:(cbk + 1) * 512],
                                      op=ALU.add), reads=[b_ps, b_xr], writes=[b_xo])
                    S.dma("sp", A("dma_start", out=dst_tiles[t], in_=xo[:]), reads=[b_xo], writes=[dst_buf])

                cur = stage1(0)
                for t in range(NT):
                    nxt = stage1(t + 1) if t + 1 < NT else None
                    stage2(t, *cur)
                    cur = nxt
            S.barrier()

        sec0 = [
            dict(kind="rope", ncols=RH * 128, ft="QrT"),
            dict(kind="rope_k", ncols=RH * 128, ft="KrT", tk="Kd"),
            dict(kind="plain", ncols=RH * 128, tk="Vr"),
            dict(kind="silu", ncols=RH * 128, tk="Rg"),
            dict(kind="normrope_q", ncols=NH * 128, ft="QnT"),
            dict(kind="plainT", ncols=NG * 128, ft="KcT"),
            dict(kind="plainT", ncols=NG * 128, ft="VcT"),
            dict(kind="normrope_k", ncols=NG * 128, ft="KsT"),
            dict(kind="plain", ncols=NG * 128, tk="Vs"),
            dict(kind="normrope_k", ncols=NG * 128, ft="KwT"),
            dict(kind="plain", ncols=NG * 128, tk="Vw"),
            dict(kind="silu", ncols=NH * 128, tk="Ng"),
            dict(kind="sigmoid", ncols=3 * NH, tk="Gl"),
        ]
        sec1 = [
            dict(kind="normrope_q", ncols=2 * DH * 128, ft="Q1T"),
            dict(kind="normrope_k", ncols=2 * DH * 128, ft="K1T"),
            dict(kind="plain", ncols=DH * 256, tk="V1"),
            dict(kind="silu", ncols=DH * 256, tk="G1"),
        ]

        def run():
            order = ["N0A0", "ret", "nsa", "C0", "N1A1", "B1", "C1"]
            i0 = order.index(start_at) if start_at else 0
            todo = order[i0:]
            if "N0A0" in todo:
                with ExitStack() as hs:
                    hT = hs.enter_context(nc.sbuf_tensor("hT0_sb", [128, 16, S_LEN], BF16))
                    hT_b = [Buf() for _ in range(NT)]
                    phase_norm(x_tiles, None, g_norm0, hT, hT_b)
                    if stop_after == "N0":
                        return
                    phase_inproj(w_in0, NC0, sec0, hT, hT_b, g_q0, g_k0, c_kdec)
                if stop_after == "A0":
                    return
            if "ret" in todo:
                phase_ret()
                if stop_after == "ret":
                    return
            if "nsa" in todo:
                phase_nsa()
                gather("Y0")
                if stop_after == "nsa":
                    return
            if "C0" in todo:
                phase_outproj("Y0", w_out0, x_tiles, None, TK["X1"], DB["X1"], D)
                if stop_after == "C0":
                    return
            if "N1A1" in todo:
                with ExitStack() as hs:
                    hT = hs.enter_context(nc.sbuf_tensor("hT1_sb", [128, 16, S_LEN], BF16))
                    hT_b = [Buf() for _ in range(NT)]
                    phase_norm(TK["X1"], DB["X1"], g_norm1, hT, hT_b)
                    phase_inproj(w_in1, NC1, sec1, hT, hT_b, g_q1, g_k1, None)
                if stop_after == "A1":
                    return
            if "B1" in todo:
                phase_diff()
                gather("Y1")
                if stop_after == "B1":
                    return
            phase_outproj("Y1", w_out1, TK["X1"], DB["X1"], out_t, out_buf, OW)

        run()
        S.barrier()
        S.emit()
    return nc


_CONST_CACHE = {}


def _consts_for(ret_heads):
    key = tuple(ret_heads)
    if key not in _CONST_CACHE:
        _CONST_CACHE[key] = make_consts(list(ret_heads))[0]
    return _CONST_CACHE[key]


def make_in_maps(inputs, split=True):
    f32 = lambda a: np.ascontiguousarray(np.asarray(a, dtype=np.float32))
    rep = lambda v: np.ascontiguousarray(np.broadcast_to(f32(v)[None, :], (128, f32(v).shape[0])))
    shared = dict(
        g_q0=rep(inputs["l0_nsa_q_norm_g"]), g_k0=rep(inputs["l0_nsa_k_norm_g"]),
        g_q1=rep(inputs["l1_q_norm_g"]), g_k1=rep(inputs["l1_k_norm_g"]),
        lam_in=np.ascontiguousarray(np.broadcast_to(
            np.stack([f32(inputs["l1_lambda_q1"]), f32(inputs["l1_lambda_k1"]),
                      f32(inputs["l1_lambda_q2"]), f32(inputs["l1_lambda_k2"])])[None], (128, 4, 128))),
        w1k=f32(inputs["l0_cmp_w1_k"]), w1v=f32(inputs["l0_cmp_w1_v"]),
        w2k=f32(inputs["l0_cmp_w2_k"]), w2v=f32(inputs["l0_cmp_w2_v"]),
        pek=np.ascontiguousarray(f32(inputs["l0_cmp_pe_k"]).T), pev=np.ascontiguousarray(f32(inputs["l0_cmp_pe_v"]).T),
    )
    x = f32(inputs["x"])
    w_in0 = f32(inputs["l0_w_in"])
    w_out0 = f32(inputs["l0_w_out"])
    w_in1 = f32(inputs["l1_w_in"])
    w_out1 = f32(inputs["l1_w_out"])
    g0 = f32(inputs["l0_norm_g"])
    g1 = f32(inputs["l1_norm_g"])
    maps = []
    if not split:
        sh = dict(shared)
        sh.update(_consts_for(range(8)))
        sh.update(w_in0=w_in0, w_out0=w_out0, w_in1=w_in1, w_out1=w_out1, g_norm0=rep(g0), g_norm1=rep(g1))
        for c in range(8):
            m = dict(sh)
            m["x"] = np.ascontiguousarray(x[c % 4])
            maps.append(m)
        return maps
    ar = np.arange
    per_hh = {}
    for hh in range(2):
        perm = np.concatenate([ar(hh * 1024, (hh + 1) * 1024), ar((1 - hh) * 1024, (2 - hh) * 1024)])
        lh = ar(4 * hh, 4 * hh + 4)
        hc = lambda base: np.concatenate([base + h * 128 + ar(128) for h in lh])
        gc = lambda base: base + hh * 128 + ar(128)
        cols0 = np.concatenate([hc(0), hc(1024), hc(2048), hc(3072), hc(4096),
                                gc(5120), gc(5376), gc(5632), gc(5888), gc(6144), gc(6400), hc(6656),
                                np.concatenate([7680 + br * 8 + lh for br in range(3)])])
        yrows0 = np.concatenate([np.concatenate([rk * 512 + ar(512), 1024 + rk * 512 + ar(512)]) for rk in range(2)])
        cols1 = np.concatenate([sec * 2048 + hh * 1024 + ar(1024) for sec in range(4)])
        d = dict(shared)
        d.update(_consts_for(range(4 * hh, 4 * hh + 4)))
        d.update(
            w_in0=np.ascontiguousarray(w_in0[perm][:, cols0]),
            w_out0=np.ascontiguousarray(w_out0[yrows0][:, perm]),
            w_in1=np.ascontiguousarray(w_in1[perm][:, cols1]),
            w_out1=np.ascontiguousarray(w_out1[:, perm[:1024]]),
            g_norm0=rep(g0[perm]), g_norm1=rep(g1[perm]),
        )
        per_hh[hh] = (d, perm)
    for c in range(8):
        b, hh = c // 2, c % 2
        d, perm = per_hh[hh]
        m = dict(d)
        m["x"] = np.ascontiguousarray(x[b][:, perm])
        yi = np.zeros((128, 2 * NT), np.int32)
        for t in range(NT):
            for rk in range(2):
                yi[:, 2 * t + rk] = (2 * b + rk) * S_LEN + t * 128 + np.arange(128)
        m["yidx"] = yi
        maps.append(m)
    return maps


_NC_CACHE = {}


def kernel(**inputs):
    if "nc" not in _NC_CACHE:
        _NC_CACHE["nc"] = build(RH=4, NH=4, NG=1, DH=4, split=True)
    nc = _NC_CACHE["nc"]
    maps = make_in_maps(inputs, split=True)
    res = run_bass_kernel_spmd(nc, maps, core_ids=list(range(8)))
    out = np.empty((4, S_LEN, D), np.float32)
    for c in range(8):
        b, hh = c // 2, c % 2
        out[b][:, hh * 1024:(hh + 1) * 1024] = np.asarray(res.results[c]["out"]).reshape(S_LEN, D // 2)
    return out
```
